# Optimizing a Trainium2 kernel written in Bass

```python
import jax, jax.numpy as jnp
from jax import lax
import numpy as np

D_MODEL = 1024
BATCH = 32
SEQ = 2048
DEPTH = 1

N_MEM = 256
EPS = 1e-6
MLSTM_HEADS = 4
MLSTM_HEAD_DIM = D_MODEL // 4
MLSTM_WIDTH = MLSTM_HEADS * MLSTM_HEAD_DIM
QKV_BLOCK = 4
CONV_WIDTH = 4
CHUNK = 64
ATTN_HEADS = 8
ATTN_HEAD_DIM = D_MODEL // 16
ATTN_WIDTH = ATTN_HEADS * ATTN_HEAD_DIM
ROPE_DIM = ATTN_HEAD_DIM // 4
ROPE_THETA = 500000.0
DILATED_PATTERNS = ((128, 1), (512, 4), (2048, 16))
BAND_BLOCK = 128
XATTN_HEADS = 4
XATTN_HEAD_DIM = D_MODEL // 8
XATTN_WIDTH = XATTN_HEADS * XATTN_HEAD_DIM

MIX_WIDTH = MLSTM_WIDTH + ATTN_WIDTH + XATTN_WIDTH
IN_SIZES = [MLSTM_WIDTH] * 3 + [ATTN_WIDTH] * 4 + [XATTN_WIDTH] * 2
IN_WIDTH = sum(IN_SIZES)
IN_OFFSETS = [int(o) for o in np.cumsum(IN_SIZES)[:-1]]

kernel_name = "hymba_mlstm_dilated_memory_block"


def rmsnorm(x, g):
    xf = x.astype(jnp.float32)
    y = xf * lax.rsqrt(jnp.mean(xf * xf, axis=-1, keepdims=True) + EPS)
    return (y * g.astype(jnp.float32)).astype(x.dtype)


def head_layernorm(h, g):
    hf = h.astype(jnp.float32)
    mu = jnp.mean(hf, axis=-1, keepdims=True)
    var = jnp.mean(jnp.square(hf - mu), axis=-1, keepdims=True)
    y = (hf - mu) * lax.rsqrt(var + EPS)
    b, s = h.shape[0], h.shape[1]
    return y.reshape(b, s, -1) * g.astype(jnp.float32)


def partial_rope(x):
    s = x.shape[1]
    half = ROPE_DIM // 2
    pos = jnp.arange(s, dtype=jnp.float32)
    inv = ROPE_THETA ** (-jnp.arange(0, ROPE_DIM, 2, dtype=jnp.float32) / ROPE_DIM)
    ang = pos[:, None] * inv[None, :]
    cos = jnp.cos(ang)[None, :, None, :]
    sin = jnp.sin(ang)[None, :, None, :]
    xf = x.astype(jnp.float32)
    x1, x2, xp = xf[..., :half], xf[..., half:ROPE_DIM], xf[..., ROPE_DIM:]
    out = jnp.concatenate([x1 * cos - x2 * sin, x2 * cos + x1 * sin, xp], axis=-1)
    return out.astype(x.dtype)


def causal_depthwise_conv(x, w, bias):
    out = lax.conv_general_dilated(
        x, w[:, None, :].astype(x.dtype), window_strides=(1,),
        padding=[(CONV_WIDTH - 1, 0)],
        dimension_numbers=('NWC', 'WIO', 'NWC'),
        feature_group_count=x.shape[-1])
    return out + bias.astype(x.dtype)


def banded_causal_attention(q, k, v, steps):
    L, hd = q.shape[-2], q.shape[-1]
    lead = q.shape[:-2]
    nb = -(-L // BAND_BLOCK)
    Lp = nb * BAND_BLOCK
    pad = [(0, 0)] * len(lead) + [(0, Lp - L), (0, 0)]
    qb, kb, vb = (jnp.pad(t.astype(jnp.float32), pad).reshape(*lead, nb, BAND_BLOCK, hd)
                  for t in (q, k, v))

    def with_prev(t):
        prev = jnp.pad(t, [(0, 0)] * len(lead) + [(1, 0), (0, 0), (0, 0)])[..., :-1, :, :]
        return jnp.concatenate([prev, t], axis=-2)

    kw, vw = with_prev(kb), with_prev(vb)
    s = jnp.einsum('...qd,...kd->...qk', qb, kw) * (hd ** -0.5)
    qi = jnp.arange(BAND_BLOCK)[:, None]
    ci = jnp.arange(2 * BAND_BLOCK)[None, :]
    dist = BAND_BLOCK + qi - ci
    key_pos = (jnp.arange(nb)[:, None, None] - 1) * BAND_BLOCK + ci[None]
    valid = (dist >= 0)[None] & (dist <= steps)[None] & (key_pos >= 0)
    s = jnp.where(valid, s, -jnp.inf)
    m = jnp.max(s, axis=-1, keepdims=True)
    p = jnp.exp(s - m)
    l = jnp.sum(p, axis=-1)
    o = jnp.einsum('...qk,...kd->...qd', p, vw) / l[..., None]
    lse = m[..., 0] + jnp.log(l)
    o = o.reshape(*lead, Lp, hd)[..., :L, :]
    lse = lse.reshape(*lead, Lp)[..., :L]
    return o, lse


def dilated_attention(q, k, v):
    b, h, s, hd = q.shape
    outs, lses = [], []
    for window, dil in DILATED_PATTERNS:
        def split(t):
            return t.reshape(b, h, s // dil, dil, hd).swapaxes(2, 3)
        o, lse = banded_causal_attention(split(q), split(k), split(v), window // dil)
        outs.append(o.swapaxes(2, 3).reshape(b, h, s, hd))
        lses.append(lse.swapaxes(2, 3).reshape(b, h, s))
    w = jax.nn.softmax(jnp.stack(lses), axis=0)
    return jnp.einsum('pbhs,pbhsd->bhsd', w, jnp.stack(outs))


def mlstm_chunkwise(q, k, v, i_pre, f_pre):
    b, s, h, dh = q.shape
    nc = s // CHUNK

    def vec_chunks(t):
        return t.astype(jnp.float32).reshape(b, nc, CHUNK, h, dh).transpose(1, 0, 3, 2, 4)

    def gate_chunks(t):
        return t.astype(jnp.float32).reshape(b, nc, CHUNK, h).transpose(1, 0, 3, 2)

    qc, kc, vc = vec_chunks(q), vec_chunks(k), vec_chunks(v)
    li = gate_chunks(i_pre)
    lf = jax.nn.log_sigmoid(gate_chunks(f_pre))
    causal = jnp.tril(jnp.ones((CHUNK, CHUNK), dtype=bool))

    def step(carry, xs):
        C, n, m_prev = carry
        qt, kt, vt, li_c, lf_c = xs
        bcum = jnp.cumsum(lf_c, axis=-1)
        dlog = bcum[..., :, None] - bcum[..., None, :] + li_c[..., None, :]
        dlog = jnp.where(causal, dlog, -jnp.inf)
        a = bcum + m_prev[..., None]
        m = jnp.maximum(a, jnp.max(dlog, axis=-1))
        wts = jnp.exp(dlog - m[..., None]) * jnp.einsum('bhtd,bhsd->bhts', qt, kt)
        inter = jnp.exp(a - m)
        num = inter[..., None] * jnp.einsum('bhtd,bhde->bhte', qt, C) + \
            jnp.einsum('bhts,bhse->bhte', wts, vt)
        den = inter * jnp.einsum('bhtd,bhd->bht', qt, n) + jnp.sum(wts, axis=-1)
        h_out = num / jnp.maximum(jnp.abs(den), jnp.exp(-m))[..., None]
        b_last = bcum[..., -1]
        g_log = b_last[..., None] - bcum + li_c
        m_new = jnp.maximum(b_last + m_prev, jnp.max(g_log, axis=-1))
        g = jnp.exp(g_log - m_new[..., None])
        decay = jnp.exp(b_last + m_prev - m_new)
        C = decay[..., None, None] * C + jnp.einsum('bhsd,bhse->bhde', g[..., None] * kt, vt)
        n = decay[..., None] * n + jnp.einsum('bhs,bhsd->bhd', g, kt)
        return (C, n, m_new), h_out

    init = (jnp.zeros((b, h, dh, dh), jnp.float32), jnp.zeros((b, h, dh), jnp.float32),
            jnp.zeros((b, h), jnp.float32))
    _, hs = lax.scan(step, init, (qc, kc, vc, li, lf))
    return hs.transpose(1, 0, 3, 2, 4).reshape(b, s, h, dh)


def block_diag_proj(t, w):
    b, s, width = t.shape
    tb = t.reshape(b, s, width // QKV_BLOCK, QKV_BLOCK)
    return jnp.einsum('bsnc,ncd->bsnd', tb, w.astype(t.dtype)).reshape(b, s, width)


def hybrid_layer(x, mem, g_norm, w_in, conv_w, conv_b, w_q_blk, w_k_blk, w_v_blk,
                 w_gate, b_gate, g_head, skip, g_mem, w_mem_kv, w_out):
    b, s, _ = x.shape
    hn = rmsnorm(x, g_norm)
    proj = hn @ w_in.astype(hn.dtype)
    xm, zm, om, qa, ka, va, za, qx, zx = jnp.split(proj, IN_OFFSETS, axis=-1)

    xc = jax.nn.silu(causal_depthwise_conv(xm, conv_w, conv_b))
    q_m = block_diag_proj(xc, w_q_blk)
    k_m = block_diag_proj(xc, w_k_blk)
    v_m = block_diag_proj(xm, w_v_blk)
    gates = jnp.concatenate([q_m, k_m, v_m], axis=-1) @ w_gate.astype(q_m.dtype) + \
        b_gate.astype(q_m.dtype)
    i_pre, f_pre = gates[..., :MLSTM_HEADS], gates[..., MLSTM_HEADS:]
    mh = lambda t: t.reshape(b, s, MLSTM_HEADS, MLSTM_HEAD_DIM)
    h_m = mlstm_chunkwise(mh(q_m), mh(k_m) * (MLSTM_HEAD_DIM ** -0.5), mh(v_m), i_pre, f_pre)
    h_m = jax.nn.sigmoid(mh(om).astype(jnp.float32)) * h_m
    y_m = (head_layernorm(h_m, g_head) + skip.astype(jnp.float32) * xc.astype(jnp.float32)) * \
        jax.nn.silu(zm.astype(jnp.float32))
    y_m = y_m.astype(x.dtype)

    ah = lambda t: t.reshape(b, s, ATTN_HEADS, ATTN_HEAD_DIM)
    qa_h = partial_rope(ah(qa)).transpose(0, 2, 1, 3)
    ka_h = partial_rope(ah(ka)).transpose(0, 2, 1, 3)
    va_h = ah(va).transpose(0, 2, 1, 3)
    o_a = dilated_attention(qa_h, ka_h, va_h)
    y_a = o_a.transpose(0, 2, 1, 3).reshape(b, s, ATTN_WIDTH).astype(x.dtype) * jax.nn.silu(za)

    mem_n = rmsnorm(mem, g_mem)
    kv = mem_n @ w_mem_kv.astype(mem_n.dtype)
    kx, vx = kv[..., :XATTN_WIDTH], kv[..., XATTN_WIDTH:]
    qx_h = qx.reshape(b, s, XATTN_HEADS, XATTN_HEAD_DIM).astype(jnp.float32)
    kx_h = kx.reshape(b, -1, XATTN_HEADS, XATTN_HEAD_DIM).astype(jnp.float32)
    vx_h = vx.reshape(b, -1, XATTN_HEADS, XATTN_HEAD_DIM).astype(jnp.float32)
    sc = jnp.einsum('bshd,bmhd->bhsm', qx_h, kx_h) * (XATTN_HEAD_DIM ** -0.5)
    p = jax.nn.softmax(sc, axis=-1)
    o_x = jnp.einsum('bhsm,bmhd->bshd', p, vx_h).reshape(b, s, XATTN_WIDTH)
    y_x = o_x.astype(x.dtype) * jax.nn.silu(zx)

    y = jnp.concatenate([y_m, y_a, y_x], axis=-1) @ w_out.astype(x.dtype)
    return x + y


def setup_inputs(seed: int = 0) -> dict:
    key = jax.random.key(seed)
    ks = jax.random.split(key, 20)
    f32 = jnp.float32
    L, D = DEPTH, D_MODEL
    nblk = MLSTM_WIDTH // QKV_BLOCK
    x = jax.random.normal(ks[0], (BATCH, SEQ, D), f32)
    mem = jax.random.normal(ks[1], (BATCH, N_MEM, D), f32)
    g_norm = 1.0 + 0.01 * jax.random.normal(ks[2], (L, D), f32)
    w_in = jax.random.normal(ks[3], (L, D, IN_WIDTH), f32) * D ** -0.5
    conv_w = jax.random.normal(ks[4], (L, CONV_WIDTH, MLSTM_WIDTH), f32) * CONV_WIDTH ** -0.5
    conv_b = 0.01 * jax.random.normal(ks[5], (L, MLSTM_WIDTH), f32)
    w_q_blk = jax.random.normal(ks[6], (L, nblk, QKV_BLOCK, QKV_BLOCK), f32) * QKV_BLOCK ** -0.5
    w_k_blk = jax.random.normal(ks[7], (L, nblk, QKV_BLOCK, QKV_BLOCK), f32) * QKV_BLOCK ** -0.5
    w_v_blk = jax.random.normal(ks[8], (L, nblk, QKV_BLOCK, QKV_BLOCK), f32) * QKV_BLOCK ** -0.5
    w_gate = jax.random.normal(ks[9], (L, 3 * MLSTM_WIDTH, 2 * MLSTM_HEADS), f32) * \
        (3 * MLSTM_WIDTH) ** -0.5
    b_gate = jnp.concatenate([
        0.1 * jax.random.normal(ks[10], (L, MLSTM_HEADS), f32),
        jax.random.uniform(ks[11], (L, MLSTM_HEADS), f32, 3.0, 6.0)], axis=-1)
    g_head = 1.0 + 0.01 * jax.random.normal(ks[12], (L, MLSTM_WIDTH), f32)
    skip = 1.0 + 0.01 * jax.random.normal(ks[13], (L, MLSTM_WIDTH), f32)
    g_mem = 1.0 + 0.01 * jax.random.normal(ks[14], (L, D), f32)
    w_mem_kv = jax.random.normal(ks[15], (L, D, 2 * XATTN_WIDTH), f32) * D ** -0.5
    w_out = jax.random.normal(ks[16], (L, MIX_WIDTH, D), f32) * MIX_WIDTH ** -0.5
    g_final = 1.0 + 0.01 * jax.random.normal(ks[17], (D,), f32)
    return {"x": x, "mem": mem, "g_norm": g_norm, "w_in": w_in, "conv_w": conv_w,
            "conv_b": conv_b, "w_q_blk": w_q_blk, "w_k_blk": w_k_blk, "w_v_blk": w_v_blk,
            "w_gate": w_gate, "b_gate": b_gate, "g_head": g_head, "skip": skip,
            "g_mem": g_mem, "w_mem_kv": w_mem_kv, "w_out": w_out, "g_final": g_final}


def reference(x, mem, g_norm, w_in, conv_w, conv_b, w_q_blk, w_k_blk, w_v_blk,
              w_gate, b_gate, g_head, skip, g_mem, w_mem_kv, w_out, g_final):
    for l in range(DEPTH):
        x = hybrid_layer(x, mem, g_norm[l], w_in[l], conv_w[l], conv_b[l], w_q_blk[l],
                         w_k_blk[l], w_v_blk[l], w_gate[l], b_gate[l], g_head[l], skip[l],
                         g_mem[l], w_mem_kv[l], w_out[l])
    return rmsnorm(x, g_final)
```

```python
import numpy as np
import ml_dtypes
from contextlib import ExitStack
import concourse.bass as bass
import concourse.mybir as mybir
from concourse.bass_utils import run_bass_kernel_spmd

F32 = mybir.dt.float32
BF16 = mybir.dt.bfloat16
AF = mybir.ActivationFunctionType
ALU = mybir.AluOpType
AX = mybir.AxisListType

import os
OPT_CDEC = os.environ.get("K_CDEC", "1") == "1"
OPT_OG = os.environ.get("K_OG", "1") == "1"
OPT_VAR = os.environ.get("K_VAR", "1") == "1"
OPT_EARLY_A = os.environ.get("K_EARLYA", "1") == "1"
OPT_LAG2 = os.environ.get("K_LAG2", "1") == "1"
OPT_SWAR = os.environ.get("K_SWAR", "0") == "1"
D = 1024
S = 2048
NCORES = 8
EPS = 1e-6
EPOCH = 30000
N_DMA_SEMS = 24


class Dep:
    __slots__ = ("w", "r", "psum")

    def __init__(self):
        self.w = None
        self.r = []
        self.psum = False


class Tile:
    def __init__(self, t, dep=None):
        self.t = t
        self.d = dep if dep is not None else Dep()

    def __getitem__(self, k):
        return self.t[k]


class View:
    def __init__(self, ap, dep):
        self.ap = ap
        self.d = dep

    def __getitem__(self, k):
        return self.ap[k]


class Eng:
    def __init__(self, fw, name, eng, n_epochs, is_pe=False):
        self.name = name
        self.eng = eng
        self.is_pe = is_pe
        self.count = 0
        self.sems = [fw.stack.enter_context(fw.nc.semaphore(f"s_{name}_{i}")) for i in range(n_epochs)]
        self.known = {}
        self.ninstr = 0


def _dep(d):
    return d if isinstance(d, Dep) else d.d


class FW:
    def __init__(self, nc):
        self.nc = nc
        self.stack = ExitStack()
        self.pe = Eng(self, "pe", nc.tensor, 3, is_pe=True)
        self.act = Eng(self, "act", nc.scalar, 2)
        self.dve = Eng(self, "dve", nc.vector, 2)
        self.pool = Eng(self, "pool", nc.gpsimd, 2)
        self.sp = Eng(self, "sp", nc.sync, 1)
        self.engs = {e.name: e for e in (self.pe, self.act, self.dve, self.pool, self.sp)}
        self.dma_sems = [self.stack.enter_context(nc.semaphore(f"s_dma_{i}")) for i in range(N_DMA_SEMS)]
        self.dma_cnt = [0] * N_DMA_SEMS
        self.dma_rr = 0
        self.sw_rr = 0
        self.nwaits = 0

    def sb(self, name, shape, dtype):
        return Tile(self.stack.enter_context(self.nc.sbuf_tensor("sb_" + name, list(shape), dtype)))

    def ps(self, name, shape, dtype):
        t = Tile(self.stack.enter_context(self.nc.psum_tensor("ps_" + name, list(shape), dtype)))
        t.d.psum = True
        return t

    def _wait(self, E, ticket):
        if ticket is None:
            return
        key, n = ticket
        if E.known.get(key, 0) >= n:
            return
        if key[0] == "E":
            src = self.engs[key[1]]
            ep = (n - 1) // EPOCH
            E.eng.wait_ge(src.sems[ep], n - ep * EPOCH)
        else:
            E.eng.wait_ge(self.dma_sems[key[1]], n)
        self.nwaits += 1
        E.known[key] = n

    def _deps(self, E, r, w):
        me = ("E", E.name)
        for d in r:
            d = _dep(d)
            t = d.w
            if t is not None and not (E.is_pe and t[0] == me):
                self._wait(E, t)
            if d.psum:
                for t in d.r:
                    if t[0] != me:
                        self._wait(E, t)
        for d in w:
            d = _dep(d)
            t = d.w
            if t is not None and not (E.is_pe and t[0] == me):
                self._wait(E, t)
            for t in d.r:
                if t[0] == me and (E.is_pe or not OPT_SWAR):
                    continue
                self._wait(E, t)

    def _commit(self, ticket, r, w):
        for d in r:
            d = _dep(d)
            d.r.append(ticket)
            if len(d.r) > 48:
                best = {}
                for k, n in d.r:
                    if best.get(k, 0) < n:
                        best[k] = n
                d.r = list(best.items())
        for d in w:
            d = _dep(d)
            d.w = ticket
            d.r = []

    def op(self, E, fn, r=(), w=(), sig=True):
        self._deps(E, r, w)
        ins = fn(E.eng)
        E.ninstr += 1
        if sig:
            E.count += 1
            ep = (E.count - 1) // EPOCH
            ins.then_inc(E.sems[ep], 1)
            ticket = (("E", E.name), E.count)
        else:
            ticket = (("E", E.name), E.count + 1)
        self._commit(ticket, r, w)
        return ticket

    def dma(self, out, in_, r=(), w=(), Q=None, **kw):
        Q = Q or self.sp
        self._deps(Q, r, w)
        if Q is self.pool:
            k = N_DMA_SEMS - 4 + self.sw_rr
            self.sw_rr = (self.sw_rr + 1) % 4
        else:
            k = self.dma_rr
            self.dma_rr = (self.dma_rr + 1) % (N_DMA_SEMS - 4)
        if self.dma_cnt[k] > 0:
            self._wait(Q, (("D", k), self.dma_cnt[k]))
        self.dma_cnt[k] += 16
        Q.eng.dma_start(out=out, in_=in_, **kw).then_inc(self.dma_sems[k], 16)
        ticket = (("D", k), self.dma_cnt[k])
        self._commit(ticket, r, w)
        return ticket

    def finish(self):
        for k in range(N_DMA_SEMS):
            if self.dma_cnt[k] > 0:
                self._wait(self.sp, (("D", k), self.dma_cnt[k]))
        for e in (self.pe, self.act, self.dve, self.pool):
            if e.count > 0:
                self._wait(self.sp, (("E", e.name), e.count))

    def close(self):
        self.stack.close()


def _consts():
    bf = ml_dtypes.bfloat16
    c = {}
    c["ident_bf"] = np.eye(128, dtype=np.float32).astype(bf)
    c["ident_f"] = np.eye(128, dtype=np.float32)
    rm = np.zeros((128, 128), np.float32)
    for hp in range(2):
        b = 64 * hp
        for m in range(8):
            rm[b + m + 8, b + m] = -1.0
        for m in range(8, 16):
            rm[b + m - 8, b + m] = 1.0
    c["rm"] = rm.astype(bf)
    pos = np.arange(S, dtype=np.float32)
    inv = (np.float32(500000.0) ** (-np.arange(0, 16, 2, dtype=np.float32) / np.float32(16))).astype(np.float32)
    ang = (pos[:, None] * inv[None, :]).astype(np.float32)
    cos = np.ones((128, S), np.float32)
    sin = np.zeros((128, S), np.float32)
    for p in range(128):
        d = p % 64
        if d < 16:
            cos[p] = np.cos(ang[:, d % 8])
            sin[p] = np.sin(ang[:, d % 8])
    c["rope"] = np.ascontiguousarray(np.stack([cos, sin], axis=1))
    k = np.arange(128)[:, None, None]
    dd = np.arange(16)[None, :, None]
    t = np.arange(128)[None, None, :]
    delta = dd * 128 + t - k
    cnt = ((delta >= 0) & (delta <= 128)).astype(np.float32) \
        + ((delta >= 0) & (delta % 4 == 0) & (delta <= 512)).astype(np.float32) \
        + ((delta >= 0) & (delta % 16 == 0) & (delta <= 2048)).astype(np.float32)
    c["msk"] = cnt.astype(bf)
    c["cm"] = (np.arange(128)[:, None] <= np.arange(128)[None, :]).astype(np.float32).astype(bf)
    sel = np.zeros((4, 4, 128), np.float32)
    for h in range(4):
        sel[h, h, :] = 1.0
    c["sel"] = sel
    return c


def _blockdiag(w):
    out = np.zeros((8, 128, 128), np.float32)
    wr = w.reshape(8, 32, 4, 4)
    for n in range(32):
        out[:, 4 * n:4 * n + 4, 4 * n:4 * n + 4] = wr[:, n]
    return out


def build(nseq=4, nblk=4, debug=False):
    nc = bass.Bass("TRN2", target_bir_lowering=False)
    fw = FW(nc)
    pe, act, dve, pool = fw.pe, fw.act, fw.dve, fw.pool

    def din(name, shape, dt=F32):
        return nc.dram_tensor(name, list(shape), dt, kind="ExternalInput").ap()

    x_d = din("x", [nseq, S, D])
    mem_d = din("mem", [nseq, 256, D])
    w_in_d = din("w_in", [D, 6144])
    w_out_d = din("w_out", [2048, D])
    w_kv_d = din("w_mem_kv", [D, 1024])
    bd_d = {n: din(n, [8, 128, 128]) for n in ("bdq", "bdk", "bdv", "bdtq", "bdtk", "bdtv")}
    wg_d = din("wg", [128, 24, 8])
    pf_d = din("pf", [128, 72])
    bg_d = din("bg", [4, 2])
    gfinal_d = din("gfinal_row", [128, D])
    cb_d = din("cb_row", [1, D])
    identbf_d = din("ident_bf", [128, 128], BF16)
    identf_d = din("ident_f", [128, 128])
    rm_d = din("rm", [128, 128], BF16)
    rope_d = din("rope", [128, 2, S])
    msk_d = din("msk", [128, 16, 128], BF16)
    cm_d = din("cm", [128, 128], BF16)
    sel_d = din("sel", [4, 4, 128])
    out_d = nc.dram_tensor("out", [nseq, S, D], F32, kind="ExternalOutput").ap()
    dbg_d = None
    if debug:
        dbg_d = nc.dram_tensor("dbg_y", [nseq, 4, 128, 16, 512], BF16, kind="ExternalOutput").ap()

    win_bf = nc.dram_tensor("win_bf", [12, 128, 8, 512], BF16, kind="Internal").ap()
    wout_bf = nc.dram_tensor("wout_bf", [4, 128, 4, 1024], BF16, kind="Internal").ap()
    wkv_bf = nc.dram_tensor("wkv_bf", [2, 128, 8, 512], BF16, kind="Internal").ap()
    win_dep = [Dep() for _ in range(12)]
    wout_dep = [Dep() for _ in range(4)]
    wkv_dep = [Dep() for _ in range(2)]

    w_in_v = w_in_d.rearrange("(kc p) (pc n) -> pc p kc n", p=128, n=512)
    order = [0, 1, 4, 2, 5, 3, 6, 7, 8, 9, 10, 11]
    for pc in order:
        fw.dma(win_bf[pc], w_in_v[pc], w=[win_dep[pc]], Q=pool)
    w_kv_v = w_kv_d.rearrange("(kc p) (pc n) -> pc p kc n", p=128, n=512)
    for pc in range(2):
        fw.dma(wkv_bf[pc], w_kv_v[pc], w=[wkv_dep[pc]], Q=pool)
    w_out_v = w_out_d.rearrange("(pc kc p) n -> pc p kc n", p=128, kc=4)
    for pc in range(4):
        fw.dma(wout_bf[pc], w_out_v[pc], w=[wout_dep[pc]], Q=pool)

    ident = fw.sb("ident", [128, 128], BF16)
    identf = fw.sb("identf", [128, 128], F32)
    rm = fw.sb("rm", [128, 128], BF16)
    msk = fw.sb("msk", [128, 16, 128], BF16)
    cm = fw.sb("cm", [128, 128], BF16)
    sel = fw.sb("sel", [4, 4, 128], F32)
    bdq = fw.sb("bdq", [128, 8, 128], BF16)
    bdk = fw.sb("bdk", [128, 8, 128], BF16)
    bdv = fw.sb("bdv", [128, 8, 128], BF16)
    convd = fw.sb("convd", [128, 32, 128], BF16)
    wfc = fw.sb("wfc", [128, 8, 8], BF16)
    wfm = fw.sb("wfm", [128, 8, 8], BF16)
    pf = fw.sb("pf", [128, 72], F32)
    gh2 = fw.sb("gh2", [128, 8], F32)
    sk2 = fw.sb("sk2", [128, 8], F32)
    bg = fw.sb("bg", [4, 2], F32)
    nbf = fw.sb("nbf", [4, 1], F32)
    gfinal_row = fw.sb("gfinal_row", [128, D], F32)
    cbrow = fw.sb("cbrow", [1, D], BF16)
    onesrow = fw.sb("onesrow", [1, 512], BF16)
    mhalf = fw.sb("mhalf", [128, 4], F32)
    ones4 = fw.sb("ones4", [4, 512], F32)

    wbuf = [fw.sb(f"wbuf{i}", [128, 8 * 512], BF16) for i in range(2)]
    xts = [fw.sb(f"xt{i}", [128, D], F32) for i in range(2)]
    xns = [fw.sb(f"xn{i}", [128, D], BF16) for i in range(2)]
    xn_i = {"i": 0}
    junk = xns[0]
    stat = fw.sb("stat", [128, 8], F32)
    hnT = fw.sb("hnT", [128, 8, 512], BF16)
    u1 = fw.sb("u1", [128, 8 * 515], BF16)
    u2 = fw.sb("u2", [128, 8 * 512], BF16)
    xmT = View(u1[:, :].rearrange("p (c t) -> p c t", c=8), u1.d)
    qTa = View(u1[:, 0:4096].rearrange("p (c h t) -> p c h t", c=4, h=2), u1.d)
    xcT = View(u2[:, :].rearrange("p (c t) -> p c t", c=8), u2.d)
    oa_tok = View(u2[:, 0:2048].rearrange("p (j f) -> p j f", j=4), u2.d)
    ox_tok = View(u2[:, 2048:4096].rearrange("p (j f) -> p j f", j=4), u2.d)
    xcarry = fw.sb("xcarry", [128, 8, 3], BF16)
    qkT = [fw.sb(f"qkT{i}", [128, 2, 2, 512], BF16) for i in range(2)]
    kgt = [fw.sb(f"kgt{i}", [128, 4, 256], BF16) for i in range(2)]
    vtok = [fw.sb(f"vtok{i}", [128, 4, 257], BF16) for i in range(2)]
    Cst = fw.sb("Cst", [128, 4, 2, 257], F32)
    Cdec = [fw.sb(f"Cdec{i}", [128, 2, 257], BF16) for i in range(2)]
    og = fw.sb("og", [128, 4, 512], BF16)
    zs = fw.sb("zs", [128, 4, 512], BF16)
    qxT = og
    zxs = zs
    zas = og
    aT = [fw.sb(f"aT{i}", [128, 128], BF16) for i in range(4)]
    h2 = [fw.sb(f"h2{i}", [128, 256], F32) for i in range(2)]
    hln = fw.sb("hln", [128, 4, 512], BF16)
    ytmps = [fw.sb(f"ytmp{i}", [128, 4, 128], BF16) for i in range(2)]
    skz = fw.sb("skz", [128, 4, 512], BF16)
    yT = fw.sb("yT", [128, 16, 512], BF16)
    kTc = fw.sb("kTc", [128, 4, S], BF16)
    kT_dep = [[Dep() for _ in range(4)] for _ in range(4)]
    vc = fw.sb("vc", [128, 16, 8, 65], BF16)
    vc_dep = [Dep() for _ in range(4)]
    NPT = 4
    pT = [fw.sb(f"pT{i}", [128, 512], BF16) for i in range(NPT)]
    ropeb = fw.sb("ropeb", [128, 2, 512], F32)
    tmpf = [fw.sb(f"tmpf{i}", [128, 512], F32) for i in range(2)]
    th_i = {"i": 0}
    xb16 = fw.sb("xb16", [128, 512], BF16)
    kxT = fw.sb("kxT", [128, 4, 256], BF16)
    vx = fw.sb("vx", [128, 2, 4, 129], BF16)
    gt = [fw.sb(f"gt{i}", [4, 512], F32) for i in range(4)]
    gsm = fw.sb("gsm", [4, 16], F32)
    gtk = fw.sb("gtk", [128, 32], F32)
    decb = fw.sb("decb", [128, 16], F32)
    rec = fw.sb("rec", [128, 8], F32)
    lnst_t = [fw.sb(f"lnst{i}", [128, 8], F32) for i in range(2)]
    lnb_t = [fw.sb(f"lnb{i}", [128, 1], F32) for i in range(2)]

    pbanks = [fw.ps(f"pb{i}", [128, 512], F32) for i in range(8)]
    pstate = {"i": 0, "pinned": set()}

    def psum(pin=False):
        while pstate["i"] in pstate["pinned"]:
            pstate["i"] = (pstate["i"] + 1) % 8
        i = pstate["i"]
        t = pbanks[i]
        pstate["i"] = (i + 1) % 8
        if pin:
            pstate["pinned"].add(i)
        return t

    def unpin(t):
        pstate["pinned"].discard(pbanks.index(t))

    def bfv(p, c):
        return p[:, :].bitcast(BF16).rearrange("p (c t) -> p c t", c=c)

    fw.dma(ident[:, :], identbf_d, w=[ident])
    fw.dma(identf[:, :], identf_d, w=[identf])
    fw.dma(rm[:, :], rm_d, w=[rm])
    fw.dma(msk[:, :, :], msk_d, w=[msk])
    fw.dma(cm[:, :], cm_d, w=[cm])
    fw.dma(sel[:, :, :], sel_d, w=[sel])
    fw.dma(pf[:, :], pf_d, w=[pf])
    fw.dma(bg[:, :], bg_d, w=[bg])
    fw.dma(gfinal_row[:, :], gfinal_d, w=[gfinal_row])
    stg = tmpf[0]
    stg3 = View(stg[:, :].rearrange("p (c f) -> p c f", c=4), stg.d)

    def load_bd(dst, src, scale=None):
        for half in range(2):
            fw.dma(stg3[:, :, :], src[half * 4:(half + 1) * 4].rearrange("c p f -> p c f"), w=[stg3])
            if scale is None:
                fw.op(dve, lambda e: e.tensor_copy(out=dst[:, half * 4:(half + 1) * 4, :], in_=stg3[:, :, :]),
                      r=[stg3], w=[dst])
            else:
                fw.op(dve, lambda e: e.tensor_scalar(out=dst[:, half * 4:(half + 1) * 4, :], in0=stg3[:, :, :],
                                                     scalar1=scale, scalar2=None, op0=ALU.mult),
                      r=[stg3], w=[dst])

    load_bd(bdq, bd_d["bdq"])
    load_bd(bdk, bd_d["bdk"], scale=1.0 / 16.0)
    load_bd(bdv, bd_d["bdv"])
    bdt = View(xns[0][:, :].rearrange("p (c f) -> p c f", c=8), xns[0].d)
    wgb = fw.sb("wgb", [128, 24, 8], BF16)
    wgst = View(tmpf[1][:, 0:192].rearrange("p (c g) -> p c g", c=24), tmpf[1].d)
    fw.dma(wgst[:, :, :], wg_d, w=[wgst])
    fw.op(dve, lambda e: e.tensor_copy(out=wgb[:, :, :], in_=wgst[:, :, :]), r=[wgst], w=[wgb])
    pfold = psum()
    pfv = View(pfold[:, 0:128].rearrange("p (a c g) -> p a c g", a=2, c=8), pfold.d)
    first = True
    for which, (nm, off) in enumerate((("bdtq", 0), ("bdtk", 8), ("bdtv", 16))):
        load_bd(bdt, bd_d[nm])
        for c in range(8):
            dst = pfv[:, 1 if nm == "bdtv" else 0, c, :]
            st = (nm == "bdtq" and c == 0)
            fw.op(pe, lambda e: e.matmul(dst, lhsT=bdt[:, c, :], rhs=wgb[:, off + c, :], start=st,
                                         stop=(nm == "bdtv" and c == 7), skip_group_check=True),
                  r=[bdt, wgb], w=[pfold])
    fw.op(dve, lambda e: e.tensor_copy(out=wfc[:, :, :], in_=pfv[:, 0, :, :]), r=[pfold], w=[wfc])
    fw.op(dve, lambda e: e.tensor_copy(out=wfm[:, :, :], in_=pfv[:, 1, :, :]), r=[pfold], w=[wfm])
    for j in range(4):
        for c in range(8):
            col = j * 8 + c
            fw.op(dve, lambda e: e.tensor_scalar(out=convd[:, col, :], in0=ident[:, :], scalar1=pf[:, col:col + 1],
                                                 scalar2=0.5, op0=ALU.mult, op1=ALU.mult), r=[ident, pf], w=[convd])
    fw.op(dve, lambda e: e.tensor_scalar(out=gh2[:, :], in0=pf[:, 40:48], scalar1=0.5, scalar2=None, op0=ALU.mult),
          r=[pf], w=[gh2])
    fw.op(dve, lambda e: e.tensor_scalar(out=sk2[:, :], in0=pf[:, 48:56], scalar1=0.5, scalar2=None, op0=ALU.mult),
          r=[pf], w=[sk2])
    cbst = View(tmpf[0][0:1, :], tmpf[0].d)
    for half in range(2):
        fw.dma(cbst[:, :], cb_d[:, half * 512:(half + 1) * 512], w=[cbst])
        fw.op(dve, lambda e: e.tensor_scalar(out=cbrow[:, half * 512:(half + 1) * 512], in0=cbst[:, :], scalar1=0.5,
                                             scalar2=None, op0=ALU.mult), r=[cbst], w=[cbrow])
    fw.op(dve, lambda e: e.memset(onesrow[:, :], 1.0), w=[onesrow])
    fw.op(dve, lambda e: e.memset(mhalf[:, :], -0.5), w=[mhalf])
    fw.op(dve, lambda e: e.memset(ones4[:, :], 1.0), w=[ones4])
    fw.op(dve, lambda e: e.tensor_scalar(out=nbf[:, :], in0=bg[:, 1:2], scalar1=-1.0, scalar2=None, op0=ALU.mult),
          r=[bg], w=[nbf])
    fw.op(dve, lambda e: e.memset(vc[:, :, :, 64:65], 1.0), w=vc_dep)
    fw.op(dve, lambda e: e.memset(vx[:, :, :, 128:129], 1.0), w=[vx])
    for i in range(2):
        fw.op(dve, lambda e: e.memset(vtok[i][:, :, 256:257], 1.0), w=[vtok[i]])

    blocks = [(s, tb) for s in range(nseq) for tb in range(nblk)]
    pieces = []
    pieces.append(("kv", 0))
    pieces.append(("kv", 1))
    for bi_, (s_, tb_) in enumerate(blocks):
        for pc in order:
            pieces.append(("in", pc))
        nxt_seq = bi_ + 1 < len(blocks) and blocks[bi_ + 1][1] == 0
        if nxt_seq and OPT_EARLY_A:
            pieces.append(("kv", 0))
            pieces.append(("kv", 1))
        for pc in range(4):
            pieces.append(("out", pc))
        if nxt_seq and not OPT_EARLY_A:
            pieces.append(("kv", 0))
            pieces.append(("kv", 1))
    wstate = {"next": 0, "slot": {}}

    def issue_piece():
        i = wstate["next"]
        if i >= len(pieces):
            return
        wstate["next"] += 1
        kind, pc = pieces[i]
        slot = wbuf[i % 2]
        if kind == "in":
            fw.dma(slot[:, :].rearrange("p (c n) -> p c n", c=8), win_bf[pc], r=[win_dep[pc]], w=[slot])
        elif kind == "kv":
            fw.dma(slot[:, :].rearrange("p (c n) -> p c n", c=8), wkv_bf[pc], r=[wkv_dep[pc]], w=[slot])
        else:
            fw.dma(slot[:, :].rearrange("p (c n) -> p c n", c=4), wout_bf[pc], r=[wout_dep[pc]], w=[slot])
        wstate["slot"][i] = slot

    pstep = {"i": 0}

    def next_piece(kind, pc):
        i = pstep["i"]
        assert pieces[i] == (kind, pc), (pieces[i], kind, pc)
        pstep["i"] += 1
        while wstate["next"] < min(len(pieces), i + 2):
            issue_piece()
        slot = wstate["slot"].pop(i)
        if kind in ("in", "kv"):
            return View(slot[:, :].rearrange("p (c n) -> p c n", c=8), slot.d)
        return View(slot[:, :].rearrange("p (c n) -> p c n", c=4), slot.d)

    def proj_fm(wv, consume):
        for fb in range(4):
            ps = psum()
            for kc in range(8):
                fw.op(pe, lambda e: e.matmul(ps[:, :], lhsT=wv[:, kc, fb * 128:(fb + 1) * 128], rhs=hnT[:, kc, :],
                                             start=(kc == 0), stop=(kc == 7)), r=[wv, hnT], w=[ps], sig=(kc == 7))
            consume(fb, ps)

    def proj_tm(wv, consume):
        for i in range(4):
            ps = psum()
            for kc in range(8):
                fw.op(pe, lambda e: e.matmul(ps[:, :], lhsT=hnT[:, kc, i * 128:(i + 1) * 128], rhs=wv[:, kc, :],
                                             start=(kc == 0), stop=(kc == 7)), r=[wv, hnT], w=[ps], sig=(kc == 7))
            consume(i, ps)

    def rms_rstd(ssq, dst):
        fw.op(pool, lambda e: e.tensor_scalar(out=dst, in0=ssq, scalar1=1.0 / D, scalar2=EPS, op0=ALU.mult,
                                              op1=ALU.add), r=[stat], w=[stat])
        fw.op(pool, lambda e: e.tensor_tensor(out=dst, in0=dst, in1=mhalf[:, 0:1], op=ALU.pow), r=[stat, mhalf],
              w=[stat])

    def norm_transpose(xt, gc0, dstT, col0):
        xn = xns[xn_i["i"] % 2]
        xn_i["i"] += 1
        junk = xn
        fw.op(act, lambda e: e.activation(out=junk[:, :], in_=xt[:, :], func=AF.Square, accum_out=stat[:, 0:1]),
              r=[xt], w=[junk, stat])
        rms_rstd(stat[:, 0:1], stat[:, 1:2])
        fw.op(dve, lambda e: e.tensor_scalar(out=xn[:, :], in0=xt[:, :], scalar1=stat[:, 1:2], scalar2=None,
                                             op0=ALU.mult), r=[xt, stat], w=[xn])
        ps = psum()
        pv = bfv(ps, 8)
        for c in range(8):
            fw.op(pe, lambda e: e.transpose(out=pv[:, c, :], in_=xn[:, c * 128:(c + 1) * 128], identity=ident[:, :]),
                  r=[xn, ident], w=[ps], sig=(c == 7))
        for c in range(8):
            fw.op(act, lambda e: e.activation(out=dstT[:, c, col0:col0 + 128], in_=pv[:, c, :], func=AF.Copy,
                                              scale=pf[:, gc0 + c:gc0 + c + 1]), r=[ps, pf], w=[dstT])

    xt_i = {"i": 0}

    def next_xt():
        t = xts[xt_i["i"] % 2]
        xt_i["i"] += 1
        return t

    def seq_setup(s):
        for i in range(2):
            xt = next_xt()
            fw.dma(xt[:, :], mem_d[s, i * 128:(i + 1) * 128, :], w=[xt])
            norm_transpose(xt, 56, hnT, i * 128)
        for pc in range(2):
            wkvb = next_piece("kv", pc)
            if pc == 0:
                for hh in range(2):
                    ps = psum()
                    for h in (2 * hh, 2 * hh + 1):
                        for kc in range(8):
                            fw.op(pe, lambda e: e.matmul(ps[:, (h % 2) * 256:(h % 2) * 256 + 256],
                                                         lhsT=wkvb[:, kc, h * 128:(h + 1) * 128],
                                                         rhs=hnT[:, kc, 0:256], start=(kc == 0), stop=(kc == 7)),
                                  r=[wkvb, hnT], w=[ps], sig=(kc == 7))
                    fw.op(dve, lambda e: e.tensor_copy(
                        out=kxT[:, 2 * hh:2 * hh + 2, :],
                        in_=ps[:, :].rearrange("p (h m) -> p h m", h=2)), r=[ps], w=[kxT])
            else:
                for mi in range(2):
                    ps = psum()
                    for kc in range(8):
                        fw.op(pe, lambda e: e.matmul(ps[:, :], lhsT=hnT[:, kc, mi * 128:(mi + 1) * 128],
                                                     rhs=wkvb[:, kc, :], start=(kc == 0), stop=(kc == 7)),
                              r=[wkvb, hnT], w=[ps], sig=(kc == 7))
                    fw.op(dve, lambda e: e.tensor_copy(out=vx[:, mi, :, 0:128],
                                                       in_=ps[:, :].rearrange("p (h d) -> p h d", h=4)),
                          r=[ps], w=[vx])
        fw.op(pool, lambda e: e.memset(Cst[:, :, :, :], 0.0), w=[Cst])
        fw.op(pool, lambda e: e.memset(xcarry[:, :, :], 0.0), w=[xcarry])
        fw.op(pool, lambda e: e.memset(gsm[:, :], 0.0), w=[gsm])

    def stage_A(s, T0):
        for i in range(4):
            xt = next_xt()
            fw.dma(xt[:, :], x_d[s, T0 + i * 128:T0 + (i + 1) * 128, :], w=[xt])
            norm_transpose(xt, 64, hnT, i * 128)
        fw.dma(ropeb[:, :, :], rope_d[:, :, T0:T0 + 512], w=[ropeb])

    for bi, (s, tb) in enumerate(blocks):
        T0 = tb * 512
        if bi == 0:
            seq_setup(s)
            stage_A(s, T0)

        fw.op(dve, lambda e: e.tensor_copy(out=xmT[:, :, 0:3], in_=xcarry[:, :, :]), r=[xcarry], w=[xmT])
        for half in range(2):
            wv = next_piece("in", half)

            def cons_xm(fb, ps, half=half):
                c = half * 4 + fb
                fw.op(act, lambda e: e.activation(out=xmT[:, c, 3:515], in_=ps[:, :], func=AF.Copy), r=[ps], w=[xmT])
            proj_fm(wv, cons_xm)
        fw.op(dve, lambda e: e.tensor_copy(out=xcarry[:, :, :], in_=xmT[:, :, 512:515]), r=[xmT], w=[xcarry])
        for c in range(8):
            ps = psum()
            for j in range(4):
                fw.op(pe, lambda e: e.matmul(ps[:, :], lhsT=convd[:, j * 8 + c, :], rhs=xmT[:, c, j:j + 512],
                                             start=(j == 0), stop=False), r=[convd, xmT], w=[ps], sig=False)
            fw.op(pe, lambda e: e.matmul(ps[:, :], lhsT=cbrow[0:1, c * 128:(c + 1) * 128], rhs=onesrow[0:1, :],
                                         start=False, stop=True), r=[cbrow, onesrow], w=[ps])
            th = tmpf[c % 2]
            fw.op(act, lambda e: e.activation(out=th[:, :], in_=ps[:, :], func=AF.Tanh), r=[ps], w=[th])
            fw.op(dve, lambda e: e.scalar_tensor_tensor(out=xcT[:, c, :], in0=th[:, :], scalar=1.0, in1=ps[:, :],
                                                        op0=ALU.add, op1=ALU.mult), r=[th, ps], w=[xcT])

        G = []
        for gi in range(2):
            ps = psum()
            for c in range(8):
                fw.op(pe, lambda e: e.matmul(ps[0:4, :], lhsT=wfc[:, c, gi * 4:(gi + 1) * 4], rhs=xcT[:, c, :],
                                             start=(c == 0), stop=False), r=[wfc, xcT], w=[ps], sig=False)
            for c in range(8):
                fw.op(pe, lambda e: e.matmul(ps[0:4, :], lhsT=wfm[:, c, gi * 4:(gi + 1) * 4], rhs=xmT[:, c, 3:515],
                                             start=False, stop=(c == 7)), r=[wfm, xmT], w=[ps], sig=(c == 7))
            G.append(ps)
        li, ff, ab, Bc = gt
        t1, ee = ab, li
        fw.op(dve, lambda e: e.tensor_scalar(out=li[:, :], in0=G[0][0:4, :], scalar1=bg[:, 0:1], scalar2=None,
                                             op0=ALU.add), r=[G[0], bg], w=[li])
        fw.op(dve, lambda e: e.tensor_scalar(out=ff[:, :], in0=G[1][0:4, :], scalar1=bg[:, 1:2], scalar2=None,
                                             op0=ALU.add), r=[G[1], bg], w=[ff])
        fw.op(act, lambda e: e.activation(out=ab[:, :], in_=ff[:, :], func=AF.Abs), r=[ff], w=[ab])
        fw.op(act, lambda e: e.activation(out=t1[:, :], in_=ab[:, :], func=AF.Exp, scale=-1.0), r=[ab], w=[t1])
        fw.op(act, lambda e: e.activation(out=t1[:, :], in_=t1[:, :], func=AF.Ln, bias=1.0), r=[t1], w=[t1])
        fw.op(dve, lambda e: e.tensor_scalar(out=ff[:, :], in0=ff[:, :], scalar1=0.0, scalar2=None, op0=ALU.min),
              r=[ff], w=[ff])
        fw.op(dve, lambda e: e.tensor_tensor(out=ff[:, :], in0=ff[:, :], in1=t1[:, :], op=ALU.subtract),
              r=[ff, t1], w=[ff])
        fw.op(dve, lambda e: e.tensor_tensor_scan(out=Bc[:, :], data0=ones4[:, :], data1=ff[:, :],
                                                  initial=gsm[:, 0:1], op0=ALU.mult, op1=ALU.add),
              r=[ones4, ff, gsm], w=[Bc])
        fw.op(dve, lambda e: e.tensor_tensor(out=ee[:, :], in0=li[:, :], in1=Bc[:, :], op=ALU.subtract),
              r=[li, Bc], w=[ee])
        fw.op(dve, lambda e: e.tensor_reduce(out=gsm[:, 4:8], in_=ee[:, :].rearrange("p (c t) -> p c t", c=4),
                                             axis=AX.X, op=ALU.max), r=[ee], w=[gsm])
        fw.op(dve, lambda e: e.tensor_tensor_scan(out=gsm[:, 8:12], data0=ones4[:, 0:4], data1=gsm[:, 4:8],
                                                  initial=gsm[:, 1:2], op0=ALU.mult, op1=ALU.max),
              r=[ones4, gsm], w=[gsm])
        fw.op(dve, lambda e: e.tensor_copy(out=gsm[:, 12:13], in_=gsm[:, 1:2]), r=[gsm], w=[gsm])
        fw.op(dve, lambda e: e.tensor_copy(out=gsm[:, 13:16], in_=gsm[:, 8:11]), r=[gsm], w=[gsm])
        fw.op(dve, lambda e: e.tensor_tensor(out=gsm[:, 12:16], in0=gsm[:, 12:16], in1=gsm[:, 8:12],
                                             op=ALU.subtract), r=[gsm], w=[gsm])
        fw.op(act, lambda e: e.activation(out=gsm[:, 12:16], in_=gsm[:, 12:16], func=AF.Exp), r=[gsm], w=[gsm])
        fw.op(dve, lambda e: e.tensor_copy(out=gsm[:, 0:1], in_=Bc[:, 511:512]), r=[Bc, gsm], w=[gsm])
        fw.op(dve, lambda e: e.tensor_copy(out=gsm[:, 1:2], in_=gsm[:, 11:12]), r=[gsm], w=[gsm])
        Rb = gsm[:, 8:12].unsqueeze(2).to_broadcast([4, 4, 128])
        fw.op(dve, lambda e: e.tensor_tensor(out=ee[:, :].rearrange("p (c t) -> p c t", c=4),
                                             in0=ee[:, :].rearrange("p (c t) -> p c t", c=4), in1=Rb,
                                             op=ALU.subtract), r=[ee, gsm], w=[ee])
        fw.op(act, lambda e: e.activation(out=ff[:, :], in_=ee[:, :], func=AF.Exp), r=[ee], w=[ff])
        fw.op(dve, lambda e: e.tensor_tensor(out=Bc[:, :].rearrange("p (c t) -> p c t", c=4),
                                             in0=Bc[:, :].rearrange("p (c t) -> p c t", c=4), in1=Rb,
                                             op=ALU.add), r=[Bc, gsm], w=[Bc])
        fw.op(act, lambda e: e.activation(out=ab[:, :], in_=Bc[:, :], func=AF.Exp, scale=-1.0), r=[Bc], w=[ab])
        def gate_tail():
            ps = psum()
            for c in range(4):
                for q, src in enumerate((ff, ab)):
                    fw.op(pe, lambda e: e.transpose(out=ps[:, c * 8 + q * 4:c * 8 + q * 4 + 4],
                                                    in_=src[0:4, c * 128:(c + 1) * 128], identity=identf[0:4, 0:4]),
                          r=[src, identf], w=[ps], sig=(c == 3 and q == 1))
            fw.op(dve, lambda e: e.tensor_copy(out=gtk[:, :], in_=ps[:, 0:32]), r=[ps], w=[gtk])
            ps = psum()
            for h in range(4):
                fw.op(pe, lambda e: e.matmul(ps[:, h * 4:(h + 1) * 4], lhsT=sel[0:4, h, :], rhs=gsm[0:4, 12:16],
                                             start=True, stop=True), r=[sel, gsm], w=[ps], sig=(h == 3))
            fw.op(dve, lambda e: e.tensor_copy(out=decb[:, :], in_=ps[:, 0:16]), r=[ps], w=[decb])

        for hh in range(2):
            wv = next_piece("in", 4 + hh)

            def cons_om(i, ps):
                fw.op(act, lambda e: e.activation(out=og[:, i, :], in_=ps[:, :], func=AF.Tanh, scale=0.5),
                      r=[ps], w=[og])
            proj_tm(wv, cons_om)
            if OPT_OG:
                fw.op(pool, lambda e: e.tensor_scalar(out=og[:, :, :], in0=og[:, :, :], scalar1=1.0, scalar2=1.0,
                                                      op0=ALU.add, op1=ALU.mult), r=[og], w=[og])
            else:
                fw.op(pool, lambda e: e.tensor_scalar(out=og[:, :, :], in0=og[:, :, :], scalar1=1.0, scalar2=None,
                                                      op0=ALU.add), r=[og], w=[og])
            wv = next_piece("in", 2 + hh)

            def cons_zm(fb, ps):
                th = tmpf[fb % 2]
                fw.op(act, lambda e: e.activation(out=th[:, :], in_=ps[:, :], func=AF.Tanh, scale=0.5),
                      r=[ps], w=[th])
                fw.op(dve, lambda e: e.scalar_tensor_tensor(out=zs[:, fb, :], in0=th[:, :], scalar=1.0, in1=ps[:, :],
                                                            op0=ALU.add, op1=ALU.mult), r=[th, ps], w=[zs])
            proj_fm(wv, cons_zm)
            for c in range(4):
                cc = 4 * hh + c
                fw.op(dve, lambda e: e.scalar_tensor_tensor(out=skz[:, c, :], in0=xcT[:, cc, :],
                                                            scalar=sk2[:, cc:cc + 1], in1=zs[:, c, :], op0=ALU.mult,
                                                            op1=ALU.mult), r=[xcT, sk2, zs], w=[skz])
                fw.op(dve, lambda e: e.tensor_scalar(out=zs[:, c, :], in0=zs[:, c, :], scalar1=gh2[:, cc:cc + 1],
                                                     scalar2=None, op0=ALU.mult), r=[zs, gh2], w=[zs])

            if hh == 0:
                gate_tail()
            for hl in range(2):
                h = 2 * hh + hl
                qk = qkT[hl]
                kg = kgt[hl]
                vt = vtok[hl]
                for qi, bdm in enumerate((bdq, bdk)):
                    for dc in range(2):
                        ch = 2 * h + dc
                        ps = psum()
                        fw.op(pe, lambda e: e.matmul(ps[:, :], lhsT=bdm[:, ch, :], rhs=xcT[:, ch, :], start=True,
                                                     stop=True), r=[bdm, xcT], w=[ps])
                        if (qi + dc) % 2 == 0:
                            fw.op(act, lambda e: e.activation(out=qk[:, qi, dc, :], in_=ps[:, :], func=AF.Copy),
                                  r=[ps], w=[qk])
                        else:
                            fw.op(dve, lambda e: e.tensor_copy(out=qk[:, qi, dc, :], in_=ps[:, :]), r=[ps], w=[qk])
                for i in range(4):
                    ps = psum()
                    for dc in range(2):
                        ch = 2 * h + dc
                        fw.op(pe, lambda e: e.matmul(ps[:, dc * 128:(dc + 1) * 128],
                                                     lhsT=xcT[:, ch, i * 128:(i + 1) * 128], rhs=bdk[:, ch, :],
                                                     start=True, stop=True), r=[xcT, bdk], w=[ps], sig=False)
                        fw.op(pe, lambda e: e.matmul(ps[:, 256 + dc * 128:256 + (dc + 1) * 128],
                                                     lhsT=xmT[:, ch, 3 + i * 128:3 + (i + 1) * 128], rhs=bdv[:, ch, :],
                                                     start=True, stop=True), r=[xmT, bdv], w=[ps], sig=(dc == 1))
                    gcol = gtk[:, i * 8 + h:i * 8 + h + 1]
                    fw.op(dve, lambda e: e.tensor_scalar(out=kg[:, i, :], in0=ps[:, 0:256], scalar1=gcol, scalar2=None,
                                                         op0=ALU.mult), r=[ps, gtk], w=[kg])
                    fw.op(act, lambda e: e.activation(out=vt[:, i, 0:256], in_=ps[:, 256:512], func=AF.Copy),
                          r=[ps], w=[vt])
            items = [(i, hl) for i in range(4) for hl in range(2)]
            live = {}

            def front(i, hl):
                h = 2 * hh + hl
                qk, kg, vt = qkT[hl], kgt[hl], vtok[hl]
                cs = slice(i * 128, (i + 1) * 128)
                gcol = gtk[:, i * 8 + h:i * 8 + h + 1]
                dcol = decb[:, h * 4 + i:h * 4 + i + 1]
                cd = Cdec[hl]
                a = aT[(i % 2) * 2 + hl]
                ps_s = psum()
                for dc in range(2):
                    fw.op(pe, lambda e: e.matmul(ps_s[:, 0:128], lhsT=qk[:, 1, dc, cs], rhs=qk[:, 0, dc, cs],
                                                 start=(dc == 0), stop=(dc == 1)), r=[qk], w=[ps_s], sig=(dc == 1))
                fw.op(dve, lambda e: e.scalar_tensor_tensor(out=a[:, :], in0=ps_s[:, 0:128], scalar=gcol,
                                                            in1=cm[:, :], op0=ALU.mult, op1=ALU.mult),
                      r=[ps_s, gtk, cm], w=[a])
                fw.op(act, lambda e: e.activation(out=cd[:, :, :], in_=Cst[:, h, :, :], func=AF.Copy, scale=dcol),
                      r=[Cst, decb], w=[cd])
                for dc in range(2):
                    ps_u = psum()
                    fw.op(pe, lambda e: e.matmul(ps_u[:, 0:257], lhsT=kg[:, i, dc * 128:(dc + 1) * 128],
                                                 rhs=vt[:, i, :], start=True, stop=True), r=[kg, vt], w=[ps_u])
                    fw.op(dve, lambda e: e.scalar_tensor_tensor(out=Cst[:, h, dc, :], in0=Cst[:, h, dc, :],
                                                                scalar=dcol, in1=ps_u[:, 0:257], op0=ALU.mult,
                                                                op1=ALU.add), r=[Cst, decb, ps_u], w=[Cst])
                ps_n = psum()
                fw.op(pe, lambda e: e.matmul(ps_n[:, 0:257], lhsT=a[:, :], rhs=vt[:, i, :], start=True,
                                             stop=False), r=[a, vt], w=[ps_n], sig=False)
                for dc in range(2):
                    fw.op(pe, lambda e: e.matmul(ps_n[:, 0:257], lhsT=qk[:, 0, dc, cs], rhs=cd[:, dc, :],
                                                 start=False, stop=(dc == 1)), r=[qk, cd], w=[ps_n],
                          sig=(dc == 1))
                live[(i, hl)] = ps_n

            def tail(i, hl):
                h = 2 * hh + hl
                ps_n = live.pop((i, hl))
                tcol = gtk[:, i * 8 + 4 + h:i * 8 + 4 + h + 1]
                hb = h2[hl]
                o = 0
                lnst = lnst_t[hl]
                rc = rec[:, hl * 4:hl * 4 + 1]
                fw.op(act, lambda e: e.activation(out=rc, in_=ps_n[:, 256:257], func=AF.Abs), r=[ps_n], w=[rec])
                fw.op(dve, lambda e: e.tensor_tensor(out=rc, in0=rc, in1=tcol, op=ALU.max), r=[rec, gtk], w=[rec])
                fw.op(dve, lambda e: e.reciprocal(out=rc, in_=rc), r=[rec], w=[rec])
                fw.op(dve, lambda e: e.scalar_tensor_tensor(out=hb[:, :], in0=ps_n[:, 0:256], scalar=rc,
                                                            in1=og[:, i, hl * 256:(hl + 1) * 256], op0=ALU.mult,
                                                            op1=ALU.mult), r=[ps_n, rec, og], w=[hb])
                st = lnst[:, o:o + 6]
                mv = lnst[:, o + 6:o + 8]
                var = lnst[:, o + 7:o + 8]
                fw.op(dve, lambda e: e.bn_stats(out=st, in_=hb[:, :]), r=[hb], w=[lnst])
                fw.op(dve, lambda e: e.bn_aggr(out=mv, in_=st), r=[lnst], w=[lnst])
                fw.op(pool, lambda e: e.tensor_scalar(out=var, in0=var, scalar1=4.0 * EPS, scalar2=1.0,
                                                      op0=ALU.add, op1=ALU.mult), r=[lnst], w=[lnst])
                fw.op(pool, lambda e: e.tensor_tensor(out=var, in0=var, in1=mhalf[:, 0:1], op=ALU.pow),
                      r=[lnst, mhalf], w=[lnst])
                lnb = lnb_t[hl]
                fw.op(pool, lambda e: e.tensor_scalar(out=lnb[:, :], in0=lnst[:, o + 6:o + 7], scalar1=var,
                                                      scalar2=-1.0, op0=ALU.mult, op1=ALU.mult),
                      r=[lnst], w=[lnb])

            def tail2(i, hl):
                hb = h2[hl]
                o = 0
                lnst = lnst_t[hl]
                lnb = lnb_t[hl]
                fw.op(act, lambda e: e.activation(out=hln[:, i, hl * 256:(hl + 1) * 256], in_=hb[:, :],
                                                  func=AF.Identity, scale=lnst[:, o + 7:o + 8], bias=lnb[:, 0:1]),
                      r=[hb, lnst, lnb], w=[hln])

            LG = 2 if OPT_LAG2 else 1
            for n in range(len(items) + LG):
                if n < len(items):
                    front(*items[n])
                if 1 <= n <= len(items):
                    tail(*items[n - 1])
                if n >= LG:
                    tail2(*items[n - LG])
            def ym_assemble(hh=hh):
                for i in range(4):
                    ps = psum()
                    pv = bfv(ps, 8)
                    ytmp = ytmps[i % 2]
                    for c in range(4):
                        fw.op(pe, lambda e: e.transpose(out=pv[:, c, :], in_=hln[:, i, c * 128:(c + 1) * 128],
                                                        identity=ident[:, :]), r=[hln, ident], w=[ps], sig=(c == 3))
                    fw.op(dve, lambda e: e.tensor_tensor(out=ytmp[:, :, :], in0=pv[:, 0:4, :],
                                                         in1=zs[:, :, i * 128:(i + 1) * 128], op=ALU.mult),
                          r=[ps, zs], w=[ytmp])
                    fw.op(pool, lambda e: e.tensor_tensor(out=yT[:, 4 * hh:4 * hh + 4, i * 128:(i + 1) * 128],
                                                          in0=ytmp[:, :, :], in1=skz[:, :, i * 128:(i + 1) * 128],
                                                          op=ALU.add), r=[ytmp, skz], w=[yT])

            if hh == 0:
                ym_assemble()
            else:
                ym_deferred = ym_assemble

        def rope_to(dsts, dst_deps, ps):
            fw.op(act, lambda e: e.activation(out=xb16[:, :], in_=ps[:, :], func=AF.Copy), r=[ps], w=[xb16])
            ps2 = psum()
            fw.op(pe, lambda e: e.matmul(ps2[:, :], lhsT=rm[:, :], rhs=xb16[:, :], start=True, stop=True),
                  r=[rm, xb16], w=[ps2])
            fw.op(dve, lambda e: e.tensor_tensor(out=tmpf[0][:, :], in0=ps[:, :], in1=ropeb[:, 0, :], op=ALU.mult),
                  r=[ps, ropeb], w=[tmpf[0]])
            fw.op(dve, lambda e: e.tensor_tensor(out=tmpf[1][:, :], in0=ps2[:, :], in1=ropeb[:, 1, :], op=ALU.mult),
                  r=[ps2, ropeb], w=[tmpf[1]])
            for (dst_ap, p0, p1) in dsts:
                fw.op(pool, lambda e: e.tensor_tensor(out=dst_ap, in0=tmpf[0][p0:p1, :], in1=tmpf[1][p0:p1, :],
                                                      op=ALU.add), r=[tmpf[0], tmpf[1]], w=dst_deps)

        fw.op(pool, lambda e: e.memset(qTa[64:128, :, 0, :], 0.0), w=[qTa])
        fw.op(pool, lambda e: e.memset(qTa[0:64, :, 1, :], 0.0), w=[qTa])
        wv = next_piece("in", 6)
        proj_fm(wv, lambda fb, ps: rope_to([(qTa[0:64, fb, 0, :], 0, 64), (qTa[64:128, fb, 1, :], 64, 128)],
                                           [qTa], ps))
        ym_deferred()
        wv = next_piece("in", 7)
        proj_fm(wv, lambda fb, ps: rope_to([(kTc[:, fb, T0:T0 + 512], 0, 128)], [kT_dep[fb][tb]], ps))
        wv = next_piece("in", 8)

        def cons_va(i, ps):
            fw.op(act, lambda e: e.activation(out=vc[:, 4 * tb + i, :, 0:64],
                                              in_=ps[:, :].rearrange("p (h d) -> p h d", h=8), func=AF.Copy),
                  r=[ps], w=[vc_dep[tb]])
        proj_tm(wv, cons_va)
        wv = next_piece("in", 9)

        def cons_za(fb, ps):
            th = tmpf[fb % 2]
            fw.op(act, lambda e: e.activation(out=th[:, :], in_=ps[:, :], func=AF.Tanh, scale=0.5), r=[ps], w=[th])
            fw.op(dve, lambda e: e.scalar_tensor_tensor(out=zas[:, fb, :], in0=th[:, :], scalar=1.0, in1=ps[:, :],
                                                        op0=ALU.add, op1=ALU.mult), r=[th, ps], w=[zas])
        proj_fm(wv, cons_za)

        pti = 0
        nI = 4 * tb + 4
        for pr in range(4):
            pos_ = [psum(pin=True), psum(pin=True)]
            povs_ = [View(p_[:, 0:260].rearrange("p (j d) -> p j d", j=4), p_.d) for p_ in pos_]
            aitems = [(I, hl) for I in range(nI) for hl in range(2)]
            alive = {}

            def a_qk(I, hl):
                hp = hl * 64
                J0 = max(I, 4 * tb)
                nq = 4 * tb + 4 - J0
                q0 = (J0 - 4 * tb) * 128
                ps_s = psum()
                fw.op(pe, lambda e: e.matmul(ps_s[:, 0:nq * 128], lhsT=kTc[:, pr, I * 128:(I + 1) * 128],
                                             rhs=qTa[:, pr, hl, q0:512], start=True, stop=True),
                      r=[kT_dep[pr][I // 4], qTa], w=[ps_s])
                alive[(I, hl)] = [ps_s, None]

            def a_exp(I, hl):
                nonlocal pti
                J0 = max(I, 4 * tb)
                nq = 4 * tb + 4 - J0
                ps_s = alive[(I, hl)][0]
                p = pT[pti % NPT]
                pti += 1
                fw.op(act, lambda e: e.activation(out=p[:, 0:nq * 128], in_=ps_s[:, 0:nq * 128], func=AF.Exp,
                                                  scale=0.125), r=[ps_s], w=[p])
                d0 = J0 - I
                meng = pool if hl == 0 else dve
                fw.op(meng, lambda e: e.tensor_tensor(out=p[:, 0:nq * 128].rearrange("p (j t) -> p j t", j=nq),
                                                      in0=p[:, 0:nq * 128].rearrange("p (j t) -> p j t", j=nq),
                                                      in1=msk[:, d0:d0 + nq, :], op=ALU.mult), r=[p, msk], w=[p])
                alive[(I, hl)][1] = p

            def a_pv(I, hl):
                h = 2 * pr + hl
                J0 = max(I, 4 * tb)
                nq = 4 * tb + 4 - J0
                p = alive.pop((I, hl))[1]
                for jj in range(nq):
                    jl = J0 + jj - 4 * tb
                    fw.op(pe, lambda e: e.matmul(povs_[hl][:, jl, :], lhsT=p[:, jj * 128:(jj + 1) * 128],
                                                 rhs=vc[:, I, h, :], start=(I == 0 and jj == 0),
                                                 stop=(I == nI - 1), skip_group_check=True),
                          r=[p, vc_dep[I // 4]], w=[pos_[hl]], sig=(jj == nq - 1))

            L1, L2 = 2, 4
            for n in range(len(aitems) + L2):
                if n < len(aitems):
                    a_qk(*aitems[n])
                if 0 <= n - L1 < len(aitems):
                    a_exp(*aitems[n - L1])
                if 0 <= n - L2 < len(aitems):
                    a_pv(*aitems[n - L2])
            for hl in range(2):
                h = 2 * pr + hl
                po, pov = pos_[hl], povs_[hl]
                fw.op(dve, lambda e: e.reciprocal(out=rec[:, 0:4], in_=pov[:, :, 64:65].rearrange("p j o -> p (j o)")),
                      r=[po], w=[rec])
                fw.op(dve, lambda e: e.tensor_tensor(out=oa_tok[:, :, h * 64:(h + 1) * 64], in0=pov[:, :, 0:64],
                                                     in1=rec[:, 0:4].unsqueeze(2).to_broadcast([128, 4, 64]),
                                                     op=ALU.mult), r=[po, rec], w=[oa_tok])
                unpin(po)
        for i in range(4):
            ps = psum()
            pv = bfv(ps, 8)
            for c in range(4):
                fw.op(pe, lambda e: e.transpose(out=pv[:, c, :], in_=oa_tok[:, i, c * 128:(c + 1) * 128],
                                                identity=ident[:, :]), r=[oa_tok, ident], w=[ps], sig=(c == 3))
            fw.op(dve, lambda e: e.scalar_tensor_tensor(out=yT[:, 8:12, i * 128:(i + 1) * 128], in0=pv[:, 0:4, :],
                                                        scalar=0.5, in1=zas[:, :, i * 128:(i + 1) * 128],
                                                        op0=ALU.mult, op1=ALU.mult), r=[ps, zas], w=[yT])

        wv = next_piece("in", 10)

        def cons_qx(fb, ps):
            fw.op(act, lambda e: e.activation(out=qxT[:, fb, :], in_=ps[:, :], func=AF.Copy), r=[ps], w=[qxT])
        proj_fm(wv, cons_qx)
        wv = next_piece("in", 11)

        def cons_zx(fb, ps):
            th = tmpf[fb % 2]
            fw.op(act, lambda e: e.activation(out=th[:, :], in_=ps[:, :], func=AF.Tanh, scale=0.5), r=[ps], w=[th])
            fw.op(dve, lambda e: e.scalar_tensor_tensor(out=zxs[:, fb, :], in0=th[:, :], scalar=1.0, in1=ps[:, :],
                                                        op0=ALU.add, op1=ALU.mult), r=[th, ps], w=[zxs])
        proj_fm(wv, cons_zx)
        xlive = {}

        def x_qk(h):
            nonlocal pti
            for mi in range(2):
                ps_s = psum()
                fw.op(pe, lambda e: e.matmul(ps_s[:, :], lhsT=kxT[:, h, mi * 128:(mi + 1) * 128], rhs=qxT[:, h, :],
                                             start=True, stop=True), r=[kxT, qxT], w=[ps_s])
                p = pT[pti % NPT]
                pti += 1
                fw.op(act, lambda e: e.activation(out=p[:, :], in_=ps_s[:, :], func=AF.Exp,
                                                  scale=float(128 ** -0.5)), r=[ps_s], w=[p])
                xlive[(h, mi)] = p

        def x_pv(h):
            pos = [psum(pin=True), psum(pin=True)]
            povs = [View(p_[:, 0:258].rearrange("p (j d) -> p j d", j=2), p_.d) for p_ in pos]
            for mi in range(2):
                p = xlive.pop((h, mi))
                for j in range(4):
                    fw.op(pe, lambda e: e.matmul(povs[j // 2][:, j % 2, :], lhsT=p[:, j * 128:(j + 1) * 128],
                                                 rhs=vx[:, mi, h, :], start=(mi == 0 and j % 2 == 0),
                                                 stop=(mi == 1), skip_group_check=True),
                          r=[p, vx], w=[pos[j // 2]], sig=(j % 2 == 1))
            for b2 in range(2):
                fw.op(dve, lambda e: e.reciprocal(out=rec[:, 4:6],
                                                  in_=povs[b2][:, :, 128:129].rearrange("p j o -> p (j o)")),
                      r=[pos[b2]], w=[rec])
                fw.op(dve, lambda e: e.tensor_tensor(out=ox_tok[:, 2 * b2:2 * b2 + 2, h * 128:(h + 1) * 128],
                                                     in0=povs[b2][:, :, 0:128],
                                                     in1=rec[:, 4:6].unsqueeze(2).to_broadcast([128, 2, 128]),
                                                     op=ALU.mult), r=[pos[b2], rec], w=[ox_tok])
                unpin(pos[b2])

        for n in range(5):
            if n < 4:
                x_qk(n)
            if n >= 1:
                x_pv(n - 1)
        for i in range(4):
            ps = psum()
            pv = bfv(ps, 8)
            for c in range(4):
                fw.op(pe, lambda e: e.transpose(out=pv[:, c, :], in_=ox_tok[:, i, c * 128:(c + 1) * 128],
                                                identity=ident[:, :]), r=[ox_tok, ident], w=[ps], sig=(c == 3))
            fw.op(dve, lambda e: e.scalar_tensor_tensor(out=yT[:, 12:16, i * 128:(i + 1) * 128], in0=pv[:, 0:4, :],
                                                        scalar=0.5, in1=zxs[:, :, i * 128:(i + 1) * 128],
                                                        op0=ALU.mult, op1=ALU.mult), r=[ps, zxs], w=[yT])
        if debug:
            fw.dma(dbg_d[s, tb], yT[:, :, :], r=[yT])

        def emit_next_A():
            if bi + 1 < len(blocks):
                ns_, ntb_ = blocks[bi + 1]
                if ntb_ == 0:
                    seq_setup(ns_)
                stage_A(ns_, ntb_ * 512)
        if OPT_EARLY_A:
            emit_next_A()
        obanks = [[psum() for _ in range(2)] for _ in range(4)]
        for q in range(4):
            wv = next_piece("out", q)
            for j in range(4):
                for half in range(2):
                    for kc in range(4):
                        c = 4 * q + kc
                        fw.op(pe, lambda e: e.matmul(obanks[j][half][:, :], lhsT=yT[:, c, j * 128:(j + 1) * 128],
                                                     rhs=wv[:, kc, half * 512:(half + 1) * 512],
                                                     start=(c == 0), stop=(c == 15)),
                              r=[yT, wv], w=[obanks[j][half]], sig=(kc == 3))
        for j in range(4):
            xt = next_xt()
            fw.dma(xt[:, :], x_d[s, T0 + j * 128:T0 + (j + 1) * 128, :], w=[xt], Q=pool)
            for half in range(2):
                fw.op(dve, lambda e: e.tensor_tensor(out=xt[:, half * 512:(half + 1) * 512],
                                                     in0=obanks[j][half][:, :],
                                                     in1=xt[:, half * 512:(half + 1) * 512], op=ALU.add),
                      r=[obanks[j][half], xt], w=[xt])
            fw.op(act, lambda e: e.activation(out=junk[:, :], in_=xt[:, :], func=AF.Square, accum_out=stat[:, 2:3]),
                  r=[xt], w=[junk, stat])
            rms_rstd(stat[:, 2:3], stat[:, 3:4])
            ot = xt
            fw.op(dve, lambda e: e.scalar_tensor_tensor(out=ot[:, :], in0=xt[:, :], scalar=stat[:, 3:4],
                                                        in1=gfinal_row[:, :], op0=ALU.mult, op1=ALU.mult),
                  r=[xt, stat, gfinal_row], w=[ot])
            fw.dma(out_d[s, T0 + j * 128:T0 + (j + 1) * 128, :], ot[:, :], r=[ot], Q=act)
        if not OPT_EARLY_A:
            emit_next_A()

    fw.finish()
    stats = dict(sbuf_free=nc.sbuf_bytes_remaining, pe=pe.ninstr, act=act.ninstr, dve=dve.ninstr, pool=pool.ninstr, waits=fw.nwaits)
    fw.close()
    return nc, stats


_CACHE = {}


def _host_inputs(inp):
    f = lambda a: np.ascontiguousarray(np.asarray(a, dtype=np.float32))
    c = _consts()
    shared = dict(c)
    shared["w_in"] = f(inp["w_in"][0])
    shared["w_out"] = f(inp["w_out"][0])
    shared["w_mem_kv"] = f(inp["w_mem_kv"][0])
    for nm, key in (("bdq", "w_q_blk"), ("bdk", "w_k_blk"), ("bdv", "w_v_blk")):
        bd = _blockdiag(f(inp[key][0]))
        shared[nm] = bd
        shared["bdt" + nm[2]] = np.ascontiguousarray(bd.transpose(0, 2, 1))
    shared["wg"] = np.ascontiguousarray(f(inp["w_gate"][0]).reshape(24, 128, 8).transpose(1, 0, 2))
    pf = np.zeros((128, 72), np.float32)
    cw = f(inp["conv_w"][0])
    for j in range(4):
        pf[:, j * 8:(j + 1) * 8] = cw[j].reshape(8, 128).T
    pf[:, 32:40] = f(inp["conv_b"][0]).reshape(8, 128).T
    pf[:, 40:48] = f(inp["g_head"][0]).reshape(8, 128).T
    pf[:, 48:56] = f(inp["skip"][0]).reshape(8, 128).T
    pf[:, 56:64] = f(inp["g_mem"][0]).reshape(8, 128).T
    pf[:, 64:72] = f(inp["g_norm"][0]).reshape(8, 128).T
    shared["pf"] = pf
    shared["bg"] = np.ascontiguousarray(f(inp["b_gate"][0]).reshape(2, 4).T)
    shared["gfinal_row"] = np.ascontiguousarray(np.broadcast_to(f(inp["g_final"])[None, :], (128, D)))
    shared["cb_row"] = f(inp["conv_b"][0]).reshape(1, D)
    return shared


def kernel(**inputs):
    x = np.asarray(inputs["x"], dtype=np.float32)
    mem = np.asarray(inputs["mem"], dtype=np.float32)
    B = x.shape[0]
    nseq = B // NCORES
    if "nc" not in _CACHE:
        _CACHE["nc"] = build(nseq=nseq, nblk=4)[0]
    nc = _CACHE["nc"]
    shared = _host_inputs(inputs)
    in_maps = []
    for c in range(NCORES):
        m = dict(shared)
        m["x"] = np.ascontiguousarray(x[c * nseq:(c + 1) * nseq])
        m["mem"] = np.ascontiguousarray(mem[c * nseq:(c + 1) * nseq])
        in_maps.append(m)
    res = run_bass_kernel_spmd(nc, in_maps, core_ids=list(range(NCORES)))
    out = np.concatenate([np.asarray(r["out"]) for r in res.results], axis=0)
    return out.astype(np.float32)
```

```python
import numpy as np
import ml_dtypes
from contextlib import ExitStack
import concourse.bass as bass
import concourse.mybir as mybir
from concourse.bass_utils import run_bass_kernel_spmd

F32 = mybir.dt.float32
BF16 = mybir.dt.bfloat16
AF = mybir.ActivationFunctionType
ALU = mybir.AluOpType
AX = mybir.AxisListType

import os
OPT_CDEC = os.environ.get("K_CDEC", "1") == "1"
OPT_OG = os.environ.get("K_OG", "1") == "1"
OPT_VAR = os.environ.get("K_VAR", "1") == "1"
OPT_EARLY_A = os.environ.get("K_EARLYA", "1") == "1"
OPT_LAG2 = os.environ.get("K_LAG2", "1") == "1"
OPT_SWAR = os.environ.get("K_SWAR", "0") == "1"
D = 1024
S = 2048
NCORES = 8
EPS = 1e-6
EPOCH = 30000
N_DMA_SEMS = 24


class Dep:
    __slots__ = ("w", "r", "psum")

    def __init__(self):
        self.w = None
        self.r = []
        self.psum = False


class Tile:
    def __init__(self, t, dep=None):
        self.t = t
        self.d = dep if dep is not None else Dep()

    def __getitem__(self, k):
        return self.t[k]


class View:
    def __init__(self, ap, dep):
        self.ap = ap
        self.d = dep

    def __getitem__(self, k):
        return self.ap[k]


class Eng:
    def __init__(self, fw, name, eng, n_epochs, is_pe=False):
        self.name = name
        self.eng = eng
        self.is_pe = is_pe
        self.count = 0
        self.sems = [fw.stack.enter_context(fw.nc.semaphore(f"s_{name}_{i}")) for i in range(n_epochs)]
        self.known = {}
        self.ninstr = 0


def _dep(d):
    return d if isinstance(d, Dep) else d.d


class FW:
    def __init__(self, nc):
        self.nc = nc
        self.stack = ExitStack()
        self.pe = Eng(self, "pe", nc.tensor, 3, is_pe=True)
        self.act = Eng(self, "act", nc.scalar, 2)
        self.dve = Eng(self, "dve", nc.vector, 2)
        self.pool = Eng(self, "pool", nc.gpsimd, 2)
        self.sp = Eng(self, "sp", nc.sync, 1)
        self.engs = {e.name: e for e in (self.pe, self.act, self.dve, self.pool, self.sp)}
        self.dma_sems = [self.stack.enter_context(nc.semaphore(f"s_dma_{i}")) for i in range(N_DMA_SEMS)]
        self.dma_cnt = [0] * N_DMA_SEMS
        self.dma_rr = 0
        self.sw_rr = 0
        self.nwaits = 0

    def sb(self, name, shape, dtype):
        return Tile(self.stack.enter_context(self.nc.sbuf_tensor("sb_" + name, list(shape), dtype)))

    def ps(self, name, shape, dtype):
        t = Tile(self.stack.enter_context(self.nc.psum_tensor("ps_" + name, list(shape), dtype)))
        t.d.psum = True
        return t

    def _wait(self, E, ticket):
        if ticket is None:
            return
        key, n = ticket
        if E.known.get(key, 0) >= n:
            return
        if key[0] == "E":
            src = self.engs[key[1]]
            ep = (n - 1) // EPOCH
            E.eng.wait_ge(src.sems[ep], n - ep * EPOCH)
        else:
            E.eng.wait_ge(self.dma_sems[key[1]], n)
        self.nwaits += 1
        E.known[key] = n

    def _deps(self, E, r, w):
        me = ("E", E.name)
        for d in r:
            d = _dep(d)
            t = d.w
            if t is not None and not (E.is_pe and t[0] == me):
                self._wait(E, t)
            if d.psum:
                for t in d.r:
                    if t[0] != me:
                        self._wait(E, t)
        for d in w:
            d = _dep(d)
            t = d.w
            if t is not None and not (E.is_pe and t[0] == me):
                self._wait(E, t)
            for t in d.r:
                if t[0] == me and (E.is_pe or not OPT_SWAR):
                    continue
                self._wait(E, t)

    def _commit(self, ticket, r, w):
        for d in r:
            d = _dep(d)
            d.r.append(ticket)
            if len(d.r) > 48:
                best = {}
                for k, n in d.r:
                    if best.get(k, 0) < n:
                        best[k] = n
                d.r = list(best.items())
        for d in w:
            d = _dep(d)
            d.w = ticket
            d.r = []

    def op(self, E, fn, r=(), w=(), sig=True):
        self._deps(E, r, w)
        ins = fn(E.eng)
        E.ninstr += 1
        if sig:
            E.count += 1
            ep = (E.count - 1) // EPOCH
            ins.then_inc(E.sems[ep], 1)
            ticket = (("E", E.name), E.count)
        else:
            ticket = (("E", E.name), E.count + 1)
        self._commit(ticket, r, w)
        return ticket

    def dma(self, out, in_, r=(), w=(), Q=None, **kw):
        Q = Q or self.sp
        self._deps(Q, r, w)
        if Q is self.pool:
            k = N_DMA_SEMS - 4 + self.sw_rr
            self.sw_rr = (self.sw_rr + 1) % 4
        else:
            k = self.dma_rr
            self.dma_rr = (self.dma_rr + 1) % (N_DMA_SEMS - 4)
        if self.dma_cnt[k] > 0:
            self._wait(Q, (("D", k), self.dma_cnt[k]))
        self.dma_cnt[k] += 16
        Q.eng.dma_start(out=out, in_=in_, **kw).then_inc(self.dma_sems[k], 16)
        ticket = (("D", k), self.dma_cnt[k])
        self._commit(ticket, r, w)
        return ticket

    def finish(self):
        for k in range(N_DMA_SEMS):
            if self.dma_cnt[k] > 0:
                self._wait(self.sp, (("D", k), self.dma_cnt[k]))
        for e in (self.pe, self.act, self.dve, self.pool):
            if e.count > 0:
                self._wait(self.sp, (("E", e.name), e.count))

    def close(self):
        self.stack.close()


def _consts():
    bf = ml_dtypes.bfloat16
    c = {}
    c["ident_bf"] = np.eye(128, dtype=np.float32).astype(bf)
    c["ident_f"] = np.eye(128, dtype=np.float32)
    rm = np.zeros((128, 128), np.float32)
    for hp in range(2):
        b = 64 * hp
        for m in range(8):
            rm[b + m + 8, b + m] = -1.0
        for m in range(8, 16):
            rm[b + m - 8, b + m] = 1.0
    c["rm"] = rm.astype(bf)
    pos = np.arange(S, dtype=np.float32)
    inv = (np.float32(500000.0) ** (-np.arange(0, 16, 2, dtype=np.float32) / np.float32(16))).astype(np.float32)
    ang = (pos[:, None] * inv[None, :]).astype(np.float32)
    cos = np.ones((128, S), np.float32)
    sin = np.zeros((128, S), np.float32)
    for p in range(128):
        d = p % 64
        if d < 16:
            cos[p] = np.cos(ang[:, d % 8])
            sin[p] = np.sin(ang[:, d % 8])
    c["rope"] = np.ascontiguousarray(np.stack([cos, sin], axis=1))
    k = np.arange(128)[:, None, None]
    dd = np.arange(16)[None, :, None]
    t = np.arange(128)[None, None, :]
    delta = dd * 128 + t - k
    cnt = ((delta >= 0) & (delta <= 128)).astype(np.float32) \
        + ((delta >= 0) & (delta % 4 == 0) & (delta <= 512)).astype(np.float32) \
        + ((delta >= 0) & (delta % 16 == 0) & (delta <= 2048)).astype(np.float32)
    c["msk"] = cnt.astype(bf)
    c["cm"] = (np.arange(128)[:, None] <= np.arange(128)[None, :]).astype(np.float32).astype(bf)
    sel = np.zeros((4, 4, 128), np.float32)
    for h in range(4):
        sel[h, h, :] = 1.0
    c["sel"] = sel
    return c


def _blockdiag(w):
    out = np.zeros((8, 128, 128), np.float32)
    wr = w.reshape(8, 32, 4, 4)
    for n in range(32):
        out[:, 4 * n:4 * n + 4, 4 * n:4 * n + 4] = wr[:, n]
    return out


def build(nseq=4, nblk=4, debug=False):
    nc = bass.Bass("TRN2", target_bir_lowering=False)
    fw = FW(nc)
    pe, act, dve, pool = fw.pe, fw.act, fw.dve, fw.pool

    def din(name, shape, dt=F32):
        return nc.dram_tensor(name, list(shape), dt, kind="ExternalInput").ap()

    x_d = din("x", [nseq, S, D])
    mem_d = din("mem", [nseq, 256, D])
    w_in_d = din("w_in", [D, 6144])
    w_out_d = din("w_out", [2048, D])
    w_kv_d = din("w_mem_kv", [D, 1024])
    bd_d = {n: din(n, [8, 128, 128]) for n in ("bdq", "bdk", "bdv", "bdtq", "bdtk", "bdtv")}
    wg_d = din("wg", [128, 24, 8])
    pf_d = din("pf", [128, 72])
    bg_d = din("bg", [4, 2])
    gfinal_d = din("gfinal_row", [128, D])
    cb_d = din("cb_row", [1, D])
    identbf_d = din("ident_bf", [128, 128], BF16)
    identf_d = din("ident_f", [128, 128])
    rm_d = din("rm", [128, 128], BF16)
    rope_d = din("rope", [128, 2, S])
    msk_d = din("msk", [128, 16, 128], BF16)
    cm_d = din("cm", [128, 128], BF16)
    sel_d = din("sel", [4, 4, 128])
    out_d = nc.dram_tensor("out", [nseq, S, D], F32, kind="ExternalOutput").ap()
    dbg_d = None
    if debug:
        dbg_d = nc.dram_tensor("dbg_y", [nseq, 4, 128, 16, 512], BF16, kind="ExternalOutput").ap()

    win_bf = nc.dram_tensor("win_bf", [12, 128, 8, 512], BF16, kind="Internal").ap()
    wout_bf = nc.dram_tensor("wout_bf", [4, 128, 4, 1024], BF16, kind="Internal").ap()
    wkv_bf = nc.dram_tensor("wkv_bf", [2, 128, 8, 512], BF16, kind="Internal").ap()
    win_dep = [Dep() for _ in range(12)]
    wout_dep = [Dep() for _ in range(4)]
    wkv_dep = [Dep() for _ in range(2)]

    w_in_v = w_in_d.rearrange("(kc p) (pc n) -> pc p kc n", p=128, n=512)
    order = [0, 1, 4, 2, 5, 3, 6, 7, 8, 9, 10, 11]
    for pc in order:
        fw.dma(win_bf[pc], w_in_v[pc], w=[win_dep[pc]], Q=pool)
    w_kv_v = w_kv_d.rearrange("(kc p) (pc n) -> pc p kc n", p=128, n=512)
    for pc in range(2):
        fw.dma(wkv_bf[pc], w_kv_v[pc], w=[wkv_dep[pc]], Q=pool)
    w_out_v = w_out_d.rearrange("(pc kc p) n -> pc p kc n", p=128, kc=4)
    for pc in range(4):
        fw.dma(wout_bf[pc], w_out_v[pc], w=[wout_dep[pc]], Q=pool)

    ident = fw.sb("ident", [128, 128], BF16)
    identf = fw.sb("identf", [128, 128], F32)
    rm = fw.sb("rm", [128, 128], BF16)
    msk = fw.sb("msk", [128, 16, 128], BF16)
    cm = fw.sb("cm", [128, 128], BF16)
    sel = fw.sb("sel", [4, 4, 128], F32)
    bdq = fw.sb("bdq", [128, 8, 128], BF16)
    bdk = fw.sb("bdk", [128, 8, 128], BF16)
    bdv = fw.sb("bdv", [128, 8, 128], BF16)
    convd = fw.sb("convd", [128, 32, 128], BF16)
    wfc = fw.sb("wfc", [128, 8, 8], BF16)
    wfm = fw.sb("wfm", [128, 8, 8], BF16)
    pf = fw.sb("pf", [128, 72], F32)
    gh2 = fw.sb("gh2", [128, 8], F32)
    sk2 = fw.sb("sk2", [128, 8], F32)
    bg = fw.sb("bg", [4, 2], F32)
    nbf = fw.sb("nbf", [4, 1], F32)
    gfinal_row = fw.sb("gfinal_row", [128, D], F32)
    cbrow = fw.sb("cbrow", [1, D], BF16)
    onesrow = fw.sb("onesrow", [1, 512], BF16)
    mhalf = fw.sb("mhalf", [128, 4], F32)
    ones4 = fw.sb("ones4", [4, 512], F32)

    wbuf = [fw.sb(f"wbuf{i}", [128, 8 * 512], BF16) for i in range(2)]
    xts = [fw.sb(f"xt{i}", [128, D], F32) for i in range(2)]
    xns = [fw.sb(f"xn{i}", [128, D], BF16) for i in range(2)]
    xn_i = {"i": 0}
    junk = xns[0]
    stat = fw.sb("stat", [128, 8], F32)
    hnT = fw.sb("hnT", [128, 8, 512], BF16)
    u1 = fw.sb("u1", [128, 8 * 515], BF16)
    u2 = fw.sb("u2", [128, 8 * 512], BF16)
    xmT = View(u1[:, :].rearrange("p (c t) -> p c t", c=8), u1.d)
    qTa = View(u1[:, 0:4096].rearrange("p (c h t) -> p c h t", c=4, h=2), u1.d)
    xcT = View(u2[:, :].rearrange("p (c t) -> p c t", c=8), u2.d)
    oa_tok = View(u2[:, 0:2048].rearrange("p (j f) -> p j f", j=4), u2.d)
    ox_tok = View(u2[:, 2048:4096].rearrange("p (j f) -> p j f", j=4), u2.d)
    xcarry = fw.sb("xcarry", [128, 8, 3], BF16)
    qkT = [fw.sb(f"qkT{i}", [128, 2, 2, 512], BF16) for i in range(2)]
    kgt = [fw.sb(f"kgt{i}", [128, 4, 256], BF16) for i in range(2)]
    vtok = [fw.sb(f"vtok{i}", [128, 4, 257], BF16) for i in range(2)]
    Cst = fw.sb("Cst", [128, 4, 2, 257], F32)
    Cdec = [fw.sb(f"Cdec{i}", [128, 2, 257], BF16) for i in range(2)]
    og = fw.sb("og", [128, 4, 512], BF16)
    zs = fw.sb("zs", [128, 4, 512], BF16)
    qxT = og
    zxs = zs
    zas = og
    aT = [fw.sb(f"aT{i}", [128, 128], BF16) for i in range(4)]
    h2 = [fw.sb(f"h2{i}", [128, 256], F32) for i in range(2)]
    hln = fw.sb("hln", [128, 4, 512], BF16)
    ytmps = [fw.sb(f"ytmp{i}", [128, 4, 128], BF16) for i in range(2)]
    skz = fw.sb("skz", [128, 4, 512], BF16)
    yT = fw.sb("yT", [128, 16, 512], BF16)
    kTc = fw.sb("kTc", [128, 4, S], BF16)
    kT_dep = [[Dep() for _ in range(4)] for _ in range(4)]
    vc = fw.sb("vc", [128, 16, 8, 65], BF16)
    vc_dep = [Dep() for _ in range(4)]
    NPT = 4
    pT = [fw.sb(f"pT{i}", [128, 512], BF16) for i in range(NPT)]
    ropeb = fw.sb("ropeb", [128, 2, 512], F32)
    tmpf = [fw.sb(f"tmpf{i}", [128, 512], F32) for i in range(2)]
    th_i = {"i": 0}
    xb16 = fw.sb("xb16", [128, 512], BF16)
    kxT = fw.sb("kxT", [128, 4, 256], BF16)
    vx = fw.sb("vx", [128, 2, 4, 129], BF16)
    gt = [fw.sb(f"gt{i}", [4, 512], F32) for i in range(4)]
    gsm = fw.sb("gsm", [4, 16], F32)
    gtk = fw.sb("gtk", [128, 32], F32)
    decb = fw.sb("decb", [128, 16], F32)
    rec = fw.sb("rec", [128, 8], F32)
    lnst_t = [fw.sb(f"lnst{i}", [128, 8], F32) for i in range(2)]
    lnb_t = [fw.sb(f"lnb{i}", [128, 1], F32) for i in range(2)]

    pbanks = [fw.ps(f"pb{i}", [128, 512], F32) for i in range(8)]
    pstate = {"i": 0, "pinned": set()}

    def psum(pin=False):
        while pstate["i"] in pstate["pinned"]:
            pstate["i"] = (pstate["i"] + 1) % 8
        i = pstate["i"]
        t = pbanks[i]
        pstate["i"] = (i + 1) % 8
        if pin:
            pstate["pinned"].add(i)
        return t

    def unpin(t):
        pstate["pinned"].discard(pbanks.index(t))

    def bfv(p, c):
        return p[:, :].bitcast(BF16).rearrange("p (c t) -> p c t", c=c)

    fw.dma(ident[:, :], identbf_d, w=[ident])
    fw.dma(identf[:, :], identf_d, w=[identf])
    fw.dma(rm[:, :], rm_d, w=[rm])
    fw.dma(msk[:, :, :], msk_d, w=[msk])
    fw.dma(cm[:, :], cm_d, w=[cm])
    fw.dma(sel[:, :, :], sel_d, w=[sel])
    fw.dma(pf[:, :], pf_d, w=[pf])
    fw.dma(bg[:, :], bg_d, w=[bg])
    fw.dma(gfinal_row[:, :], gfinal_d, w=[gfinal_row])
    stg = tmpf[0]
    stg3 = View(stg[:, :].rearrange("p (c f) -> p c f", c=4), stg.d)

    def load_bd(dst, src, scale=None):
        for half in range(2):
            fw.dma(stg3[:, :, :], src[half * 4:(half + 1) * 4].rearrange("c p f -> p c f"), w=[stg3])
            if scale is None:
                fw.op(dve, lambda e: e.tensor_copy(out=dst[:, half * 4:(half + 1) * 4, :], in_=stg3[:, :, :]),
                      r=[stg3], w=[dst])
            else:
                fw.op(dve, lambda e: e.tensor_scalar(out=dst[:, half * 4:(half + 1) * 4, :], in0=stg3[:, :, :],
                                                     scalar1=scale, scalar2=None, op0=ALU.mult),
                      r=[stg3], w=[dst])

    load_bd(bdq, bd_d["bdq"])
    load_bd(bdk, bd_d["bdk"], scale=1.0 / 16.0)
    load_bd(bdv, bd_d["bdv"])
    bdt = View(xns[0][:, :].rearrange("p (c f) -> p c f", c=8), xns[0].d)
    wgb = fw.sb("wgb", [128, 24, 8], BF16)
    wgst = View(tmpf[1][:, 0:192].rearrange("p (c g) -> p c g", c=24), tmpf[1].d)
    fw.dma(wgst[:, :, :], wg_d, w=[wgst])
    fw.op(dve, lambda e: e.tensor_copy(out=wgb[:, :, :], in_=wgst[:, :, :]), r=[wgst], w=[wgb])
    pfold = psum()
    pfv = View(pfold[:, 0:128].rearrange("p (a c g) -> p a c g", a=2, c=8), pfold.d)
    first = True
    for which, (nm, off) in enumerate((("bdtq", 0), ("bdtk", 8), ("bdtv", 16))):
        load_bd(bdt, bd_d[nm])
        for c in range(8):
            dst = pfv[:, 1 if nm == "bdtv" else 0, c, :]
            st = (nm == "bdtq" and c == 0)
            fw.op(pe, lambda e: e.matmul(dst, lhsT=bdt[:, c, :], rhs=wgb[:, off + c, :], start=st,
                                         stop=(nm == "bdtv" and c == 7), skip_group_check=True),
                  r=[bdt, wgb], w=[pfold])
    fw.op(dve, lambda e: e.tensor_copy(out=wfc[:, :, :], in_=pfv[:, 0, :, :]), r=[pfold], w=[wfc])
    fw.op(dve, lambda e: e.tensor_copy(out=wfm[:, :, :], in_=pfv[:, 1, :, :]), r=[pfold], w=[wfm])
    for j in range(4):
        for c in range(8):
            col = j * 8 + c
            fw.op(dve, lambda e: e.tensor_scalar(out=convd[:, col, :], in0=ident[:, :], scalar1=pf[:, col:col + 1],
                                                 scalar2=0.5, op0=ALU.mult, op1=ALU.mult), r=[ident, pf], w=[convd])
    fw.op(dve, lambda e: e.tensor_scalar(out=gh2[:, :], in0=pf[:, 40:48], scalar1=0.5, scalar2=None, op0=ALU.mult),
          r=[pf], w=[gh2])
    fw.op(dve, lambda e: e.tensor_scalar(out=sk2[:, :], in0=pf[:, 48:56], scalar1=0.5, scalar2=None, op0=ALU.mult),
          r=[pf], w=[sk2])
    cbst = View(tmpf[0][0:1, :], tmpf[0].d)
    for half in range(2):
        fw.dma(cbst[:, :], cb_d[:, half * 512:(half + 1) * 512], w=[cbst])
        fw.op(dve, lambda e: e.tensor_scalar(out=cbrow[:, half * 512:(half + 1) * 512], in0=cbst[:, :], scalar1=0.5,
                                             scalar2=None, op0=ALU.mult), r=[cbst], w=[cbrow])
    fw.op(dve, lambda e: e.memset(onesrow[:, :], 1.0), w=[onesrow])
    fw.op(dve, lambda e: e.memset(mhalf[:, :], -0.5), w=[mhalf])
    fw.op(dve, lambda e: e.memset(ones4[:, :], 1.0), w=[ones4])
    fw.op(dve, lambda e: e.tensor_scalar(out=nbf[:, :], in0=bg[:, 1:2], scalar1=-1.0, scalar2=None, op0=ALU.mult),
          r=[bg], w=[nbf])
    fw.op(dve, lambda e: e.memset(vc[:, :, :, 64:65], 1.0), w=vc_dep)
    fw.op(dve, lambda e: e.memset(vx[:, :, :, 128:129], 1.0), w=[vx])
    for i in range(2):
        fw.op(dve, lambda e: e.memset(vtok[i][:, :, 256:257], 1.0), w=[vtok[i]])

    blocks = [(s, tb) for s in range(nseq) for tb in range(nblk)]
    pieces = []
    pieces.append(("kv", 0))
    pieces.append(("kv", 1))
    for bi_, (s_, tb_) in enumerate(blocks):
        for pc in order:
            pieces.append(("in", pc))
        nxt_seq = bi_ + 1 < len(blocks) and blocks[bi_ + 1][1] == 0
        if nxt_seq and OPT_EARLY_A:
            pieces.append(("kv", 0))
            pieces.append(("kv", 1))
        for pc in range(4):
            pieces.append(("out", pc))
        if nxt_seq and not OPT_EARLY_A:
            pieces.append(("kv", 0))
            pieces.append(("kv", 1))
    wstate = {"next": 0, "slot": {}}

    def issue_piece():
        i = wstate["next"]
        if i >= len(pieces):
            return
        wstate["next"] += 1
        kind, pc = pieces[i]
        slot = wbuf[i % 2]
        if kind == "in":
            fw.dma(slot[:, :].rearrange("p (c n) -> p c n", c=8), win_bf[pc], r=[win_dep[pc]], w=[slot])
        elif kind == "kv":
            fw.dma(slot[:, :].rearrange("p (c n) -> p c n", c=8), wkv_bf[pc], r=[wkv_dep[pc]], w=[slot])
        else:
            fw.dma(slot[:, :].rearrange("p (c n) -> p c n", c=4), wout_bf[pc], r=[wout_dep[pc]], w=[slot])
        wstate["slot"][i] = slot

    pstep = {"i": 0}

    def next_piece(kind, pc):
        i = pstep["i"]
        assert pieces[i] == (kind, pc), (pieces[i], kind, pc)
        pstep["i"] += 1
        while wstate["next"] < min(len(pieces), i + 2):
            issue_piece()
        slot = wstate["slot"].pop(i)
        if kind in ("in", "kv"):
            return View(slot[:, :].rearrange("p (c n) -> p c n", c=8), slot.d)
        return View(slot[:, :].rearrange("p (c n) -> p c n", c=4), slot.d)

    def proj_fm(wv, consume):
        for fb in range(4):
            ps = psum()
            for kc in range(8):
                fw.op(pe, lambda e: e.matmul(ps[:, :], lhsT=wv[:, kc, fb * 128:(fb + 1) * 128], rhs=hnT[:, kc, :],
                                             start=(kc == 0), stop=(kc == 7)), r=[wv, hnT], w=[ps], sig=(kc == 7))
            consume(fb, ps)

    def proj_tm(wv, consume):
        for i in range(4):
            ps = psum()
            for kc in range(8):
                fw.op(pe, lambda e: e.matmul(ps[:, :], lhsT=hnT[:, kc, i * 128:(i + 1) * 128], rhs=wv[:, kc, :],
                                             start=(kc == 0), stop=(kc == 7)), r=[wv, hnT], w=[ps], sig=(kc == 7))
            consume(i, ps)

    def rms_rstd(ssq, dst):
        fw.op(pool, lambda e: e.tensor_scalar(out=dst, in0=ssq, scalar1=1.0 / D, scalar2=EPS, op0=ALU.mult,
                                              op1=ALU.add), r=[stat], w=[stat])
        fw.op(pool, lambda e: e.tensor_tensor(out=dst, in0=dst, in1=mhalf[:, 0:1], op=ALU.pow), r=[stat, mhalf],
              w=[stat])

    def norm_transpose(xt, gc0, dstT, col0):
        xn = xns[xn_i["i"] % 2]
        xn_i["i"] += 1
        junk = xn
        fw.op(act, lambda e: e.activation(out=junk[:, :], in_=xt[:, :], func=AF.Square, accum_out=stat[:, 0:1]),
              r=[xt], w=[junk, stat])
        rms_rstd(stat[:, 0:1], stat[:, 1:2])
        fw.op(dve, lambda e: e.tensor_scalar(out=xn[:, :], in0=xt[:, :], scalar1=stat[:, 1:2], scalar2=None,
                                             op0=ALU.mult), r=[xt, stat], w=[xn])
        ps = psum()
        pv = bfv(ps, 8)
        for c in range(8):
            fw.op(pe, lambda e: e.transpose(out=pv[:, c, :], in_=xn[:, c * 128:(c + 1) * 128], identity=ident[:, :]),
                  r=[xn, ident], w=[ps], sig=(c == 7))
        for c in range(8):
            fw.op(act, lambda e: e.activation(out=dstT[:, c, col0:col0 + 128], in_=pv[:, c, :], func=AF.Copy,
                                              scale=pf[:, gc0 + c:gc0 + c + 1]), r=[ps, pf], w=[dstT])

    xt_i = {"i": 0}

    def next_xt():
        t = xts[xt_i["i"] % 2]
        xt_i["i"] += 1
        return t

    def seq_setup(s):
        for i in range(2):
            xt = next_xt()
            fw.dma(xt[:, :], mem_d[s, i * 128:(i + 1) * 128, :], w=[xt])
            norm_transpose(xt, 56, hnT, i * 128)
        for pc in range(2):
            wkvb = next_piece("kv", pc)
            if pc == 0:
                for hh in range(2):
                    ps = psum()
                    for h in (2 * hh, 2 * hh + 1):
                        for kc in range(8):
                            fw.op(pe, lambda e: e.matmul(ps[:, (h % 2) * 256:(h % 2) * 256 + 256],
                                                         lhsT=wkvb[:, kc, h * 128:(h + 1) * 128],
                                                         rhs=hnT[:, kc, 0:256], start=(kc == 0), stop=(kc == 7)),
                                  r=[wkvb, hnT], w=[ps], sig=(kc == 7))
                    fw.op(dve, lambda e: e.tensor_copy(
                        out=kxT[:, 2 * hh:2 * hh + 2, :],
                        in_=ps[:, :].rearrange("p (h m) -> p h m", h=2)), r=[ps], w=[kxT])
            else:
                for mi in range(2):
                    ps = psum()
                    for kc in range(8):
                        fw.op(pe, lambda e: e.matmul(ps[:, :], lhsT=hnT[:, kc, mi * 128:(mi + 1) * 128],
                                                     rhs=wkvb[:, kc, :], start=(kc == 0), stop=(kc == 7)),
                              r=[wkvb, hnT], w=[ps], sig=(kc == 7))
                    fw.op(dve, lambda e: e.tensor_copy(out=vx[:, mi, :, 0:128],
                                                       in_=ps[:, :].rearrange("p (h d) -> p h d", h=4)),
                          r=[ps], w=[vx])
        fw.op(pool, lambda e: e.memset(Cst[:, :, :, :], 0.0), w=[Cst])
        fw.op(pool, lambda e: e.memset(xcarry[:, :, :], 0.0), w=[xcarry])
        fw.op(pool, lambda e: e.memset(gsm[:, :], 0.0), w=[gsm])

    def stage_A(s, T0):
        for i in range(4):
            xt = next_xt()
            fw.dma(xt[:, :], x_d[s, T0 + i * 128:T0 + (i + 1) * 128, :], w=[xt])
            norm_transpose(xt, 64, hnT, i * 128)
        fw.dma(ropeb[:, :, :], rope_d[:, :, T0:T0 + 512], w=[ropeb])

    for bi, (s, tb) in enumerate(blocks):
        T0 = tb * 512
        if bi == 0:
            seq_setup(s)
            stage_A(s, T0)

        fw.op(dve, lambda e: e.tensor_copy(out=xmT[:, :, 0:3], in_=xcarry[:, :, :]), r=[xcarry], w=[xmT])
        for half in range(2):
            wv = next_piece("in", half)

            def cons_xm(fb, ps, half=half):
                c = half * 4 + fb
                fw.op(act, lambda e: e.activation(out=xmT[:, c, 3:515], in_=ps[:, :], func=AF.Copy), r=[ps], w=[xmT])
            proj_fm(wv, cons_xm)
        fw.op(dve, lambda e: e.tensor_copy(out=xcarry[:, :, :], in_=xmT[:, :, 512:515]), r=[xmT], w=[xcarry])
        for c in range(8):
            ps = psum()
            for j in range(4):
                fw.op(pe, lambda e: e.matmul(ps[:, :], lhsT=convd[:, j * 8 + c, :], rhs=xmT[:, c, j:j + 512],
                                             start=(j == 0), stop=False), r=[convd, xmT], w=[ps], sig=False)
            fw.op(pe, lambda e: e.matmul(ps[:, :], lhsT=cbrow[0:1, c * 128:(c + 1) * 128], rhs=onesrow[0:1, :],
                                         start=False, stop=True), r=[cbrow, onesrow], w=[ps])
            th = tmpf[c % 2]
            fw.op(act, lambda e: e.activation(out=th[:, :], in_=ps[:, :], func=AF.Tanh), r=[ps], w=[th])
            fw.op(dve, lambda e: e.scalar_tensor_tensor(out=xcT[:, c, :], in0=th[:, :], scalar=1.0, in1=ps[:, :],
                                                        op0=ALU.add, op1=ALU.mult), r=[th, ps], w=[xcT])

        G = []
        for gi in range(2):
            ps = psum()
            for c in range(8):
                fw.op(pe, lambda e: e.matmul(ps[0:4, :], lhsT=wfc[:, c, gi * 4:(gi + 1) * 4], rhs=xcT[:, c, :],
                                             start=(c == 0), stop=False), r=[wfc, xcT], w=[ps], sig=False)
            for c in range(8):
                fw.op(pe, lambda e: e.matmul(ps[0:4, :], lhsT=wfm[:, c, gi * 4:(gi + 1) * 4], rhs=xmT[:, c, 3:515],
                                             start=False, stop=(c == 7)), r=[wfm, xmT], w=[ps], sig=(c == 7))
            G.append(ps)
        li, ff, ab, Bc = gt
        t1, ee = ab, li
        fw.op(dve, lambda e: e.tensor_scalar(out=li[:, :], in0=G[0][0:4, :], scalar1=bg[:, 0:1], scalar2=None,
                                             op0=ALU.add), r=[G[0], bg], w=[li])
        fw.op(dve, lambda e: e.tensor_scalar(out=ff[:, :], in0=G[1][0:4, :], scalar1=bg[:, 1:2], scalar2=None,
                                             op0=ALU.add), r=[G[1], bg], w=[ff])
        fw.op(act, lambda e: e.activation(out=ab[:, :], in_=ff[:, :], func=AF.Abs), r=[ff], w=[ab])
        fw.op(act, lambda e: e.activation(out=t1[:, :], in_=ab[:, :], func=AF.Exp, scale=-1.0), r=[ab], w=[t1])
        fw.op(act, lambda e: e.activation(out=t1[:, :], in_=t1[:, :], func=AF.Ln, bias=1.0), r=[t1], w=[t1])
        fw.op(dve, lambda e: e.tensor_scalar(out=ff[:, :], in0=ff[:, :], scalar1=0.0, scalar2=None, op0=ALU.min),
              r=[ff], w=[ff])
        fw.op(dve, lambda e: e.tensor_tensor(out=ff[:, :], in0=ff[:, :], in1=t1[:, :], op=ALU.subtract),
              r=[ff, t1], w=[ff])
        fw.op(dve, lambda e: e.tensor_tensor_scan(out=Bc[:, :], data0=ones4[:, :], data1=ff[:, :],
                                                  initial=gsm[:, 0:1], op0=ALU.mult, op1=ALU.add),
              r=[ones4, ff, gsm], w=[Bc])
        fw.op(dve, lambda e: e.tensor_tensor(out=ee[:, :], in0=li[:, :], in1=Bc[:, :], op=ALU.subtract),
              r=[li, Bc], w=[ee])
        fw.op(dve, lambda e: e.tensor_reduce(out=gsm[:, 4:8], in_=ee[:, :].rearrange("p (c t) -> p c t", c=4),
                                             axis=AX.X, op=ALU.max), r=[ee], w=[gsm])
        fw.op(dve, lambda e: e.tensor_tensor_scan(out=gsm[:, 8:12], data0=ones4[:, 0:4], data1=gsm[:, 4:8],
                                                  initial=gsm[:, 1:2], op0=ALU.mult, op1=ALU.max),
              r=[ones4, gsm], w=[gsm])
        fw.op(dve, lambda e: e.tensor_copy(out=gsm[:, 12:13], in_=gsm[:, 1:2]), r=[gsm], w=[gsm])
        fw.op(dve, lambda e: e.tensor_copy(out=gsm[:, 13:16], in_=gsm[:, 8:11]), r=[gsm], w=[gsm])
        fw.op(dve, lambda e: e.tensor_tensor(out=gsm[:, 12:16], in0=gsm[:, 12:16], in1=gsm[:, 8:12],
                                             op=ALU.subtract), r=[gsm], w=[gsm])
        fw.op(act, lambda e: e.activation(out=gsm[:, 12:16], in_=gsm[:, 12:16], func=AF.Exp), r=[gsm], w=[gsm])
        fw.op(dve, lambda e: e.tensor_copy(out=gsm[:, 0:1], in_=Bc[:, 511:512]), r=[Bc, gsm], w=[gsm])
        fw.op(dve, lambda e: e.tensor_copy(out=gsm[:, 1:2], in_=gsm[:, 11:12]), r=[gsm], w=[gsm])
        Rb = gsm[:, 8:12].unsqueeze(2).to_broadcast([4, 4, 128])
        fw.op(dve, lambda e: e.tensor_tensor(out=ee[:, :].rearrange("p (c t) -> p c t", c=4),
                                             in0=ee[:, :].rearrange("p (c t) -> p c t", c=4), in1=Rb,
                                             op=ALU.subtract), r=[ee, gsm], w=[ee])
        fw.op(act, lambda e: e.activation(out=ff[:, :], in_=ee[:, :], func=AF.Exp), r=[ee], w=[ff])
        fw.op(dve, lambda e: e.tensor_tensor(out=Bc[:, :].rearrange("p (c t) -> p c t", c=4),
                                             in0=Bc[:, :].rearrange("p (c t) -> p c t", c=4), in1=Rb,
                                             op=ALU.add), r=[Bc, gsm], w=[Bc])
        fw.op(act, lambda e: e.activation(out=ab[:, :], in_=Bc[:, :], func=AF.Exp, scale=-1.0), r=[Bc], w=[ab])
        def gate_tail():
            ps = psum()
            for c in range(4):
                for q, src in enumerate((ff, ab)):
                    fw.op(pe, lambda e: e.transpose(out=ps[:, c * 8 + q * 4:c * 8 + q * 4 + 4],
                                                    in_=src[0:4, c * 128:(c + 1) * 128], identity=identf[0:4, 0:4]),
                          r=[src, identf], w=[ps], sig=(c == 3 and q == 1))
            fw.op(dve, lambda e: e.tensor_copy(out=gtk[:, :], in_=ps[:, 0:32]), r=[ps], w=[gtk])
            ps = psum()
            for h in range(4):
                fw.op(pe, lambda e: e.matmul(ps[:, h * 4:(h + 1) * 4], lhsT=sel[0:4, h, :], rhs=gsm[0:4, 12:16],
                                             start=True, stop=True), r=[sel, gsm], w=[ps], sig=(h == 3))
            fw.op(dve, lambda e: e.tensor_copy(out=decb[:, :], in_=ps[:, 0:16]), r=[ps], w=[decb])

        for hh in range(2):
            wv = next_piece("in", 4 + hh)

            def cons_om(i, ps):
                fw.op(act, lambda e: e.activation(out=og[:, i, :], in_=ps[:, :], func=AF.Tanh, scale=0.5),
                      r=[ps], w=[og])
            proj_tm(wv, cons_om)
            if OPT_OG:
                fw.op(pool, lambda e: e.tensor_scalar(out=og[:, :, :], in0=og[:, :, :], scalar1=1.0, scalar2=1.0,
                                                      op0=ALU.add, op1=ALU.mult), r=[og], w=[og])
            else:
                fw.op(pool, lambda e: e.tensor_scalar(out=og[:, :, :], in0=og[:, :, :], scalar1=1.0, scalar2=None,
                                                      op0=ALU.add), r=[og], w=[og])
            wv = next_piece("in", 2 + hh)

            def cons_zm(fb, ps):
                th = tmpf[fb % 2]
                fw.op(act, lambda e: e.activation(out=th[:, :], in_=ps[:, :], func=AF.Tanh, scale=0.5),
                      r=[ps], w=[th])
                fw.op(dve, lambda e: e.scalar_tensor_tensor(out=zs[:, fb, :], in0=th[:, :], scalar=1.0, in1=ps[:, :],
                                                            op0=ALU.add, op1=ALU.mult), r=[th, ps], w=[zs])
            proj_fm(wv, cons_zm)
            for c in range(4):
                cc = 4 * hh + c
                fw.op(dve, lambda e: e.scalar_tensor_tensor(out=skz[:, c, :], in0=xcT[:, cc, :],
                                                            scalar=sk2[:, cc:cc + 1], in1=zs[:, c, :], op0=ALU.mult,
                                                            op1=ALU.mult), r=[xcT, sk2, zs], w=[skz])
                fw.op(dve, lambda e: e.tensor_scalar(out=zs[:, c, :], in0=zs[:, c, :], scalar1=gh2[:, cc:cc + 1],
                                                     scalar2=None, op0=ALU.mult), r=[zs, gh2], w=[zs])

            if hh == 0:
                gate_tail()
            for hl in range(2):
                h = 2 * hh + hl
                qk = qkT[hl]
                kg = kgt[hl]
                vt = vtok[hl]
                for qi, bdm in enumerate((bdq, bdk)):
                    for dc in range(2):
                        ch = 2 * h + dc
                        ps = psum()
                        fw.op(pe, lambda e: e.matmul(ps[:, :], lhsT=bdm[:, ch, :], rhs=xcT[:, ch, :], start=True,
                                                     stop=True), r=[bdm, xcT], w=[ps])
                        if (qi + dc) % 2 == 0:
                            fw.op(act, lambda e: e.activation(out=qk[:, qi, dc, :], in_=ps[:, :], func=AF.Copy),
                                  r=[ps], w=[qk])
                        else:
                            fw.op(dve, lambda e: e.tensor_copy(out=qk[:, qi, dc, :], in_=ps[:, :]), r=[ps], w=[qk])
                for i in range(4):
                    ps = psum()
                    for dc in range(2):
                        ch = 2 * h + dc
                        fw.op(pe, lambda e: e.matmul(ps[:, dc * 128:(dc + 1) * 128],
                                                     lhsT=xcT[:, ch, i * 128:(i + 1) * 128], rhs=bdk[:, ch, :],
                                                     start=True, stop=True), r=[xcT, bdk], w=[ps], sig=False)
                        fw.op(pe, lambda e: e.matmul(ps[:, 256 + dc * 128:256 + (dc + 1) * 128],
                                                     lhsT=xmT[:, ch, 3 + i * 128:3 + (i + 1) * 128], rhs=bdv[:, ch, :],
                                                     start=True, stop=True), r=[xmT, bdv], w=[ps], sig=(dc == 1))
                    gcol = gtk[:, i * 8 + h:i * 8 + h + 1]
                    fw.op(dve, lambda e: e.tensor_scalar(out=kg[:, i, :], in0=ps[:, 0:256], scalar1=gcol, scalar2=None,
                                                         op0=ALU.mult), r=[ps, gtk], w=[kg])
                    fw.op(act, lambda e: e.activation(out=vt[:, i, 0:256], in_=ps[:, 256:512], func=AF.Copy),
                          r=[ps], w=[vt])
            items = [(i, hl) for i in range(4) for hl in range(2)]
            live = {}

            def front(i, hl):
                h = 2 * hh + hl
                qk, kg, vt = qkT[hl], kgt[hl], vtok[hl]
                cs = slice(i * 128, (i + 1) * 128)
                gcol = gtk[:, i * 8 + h:i * 8 + h + 1]
                dcol = decb[:, h * 4 + i:h * 4 + i + 1]
                cd = Cdec[hl]
                a = aT[(i % 2) * 2 + hl]
                ps_s = psum()
                for dc in range(2):
                    fw.op(pe, lambda e: e.matmul(ps_s[:, 0:128], lhsT=qk[:, 1, dc, cs], rhs=qk[:, 0, dc, cs],
                                                 start=(dc == 0), stop=(dc == 1)), r=[qk], w=[ps_s], sig=(dc == 1))
                fw.op(dve, lambda e: e.scalar_tensor_tensor(out=a[:, :], in0=ps_s[:, 0:128], scalar=gcol,
                                                            in1=cm[:, :], op0=ALU.mult, op1=ALU.mult),
                      r=[ps_s, gtk, cm], w=[a])
                fw.op(act, lambda e: e.activation(out=cd[:, :, :], in_=Cst[:, h, :, :], func=AF.Copy, scale=dcol),
                      r=[Cst, decb], w=[cd])
                for dc in range(2):
                    ps_u = psum()
                    fw.op(pe, lambda e: e.matmul(ps_u[:, 0:257], lhsT=kg[:, i, dc * 128:(dc + 1) * 128],
                                                 rhs=vt[:, i, :], start=True, stop=True), r=[kg, vt], w=[ps_u])
                    fw.op(dve, lambda e: e.scalar_tensor_tensor(out=Cst[:, h, dc, :], in0=Cst[:, h, dc, :],
                                                                scalar=dcol, in1=ps_u[:, 0:257], op0=ALU.mult,
                                                                op1=ALU.add), r=[Cst, decb, ps_u], w=[Cst])
                ps_n = psum()
                fw.op(pe, lambda e: e.matmul(ps_n[:, 0:257], lhsT=a[:, :], rhs=vt[:, i, :], start=True,
                                             stop=False), r=[a, vt], w=[ps_n], sig=False)
                for dc in range(2):
                    fw.op(pe, lambda e: e.matmul(ps_n[:, 0:257], lhsT=qk[:, 0, dc, cs], rhs=cd[:, dc, :],
                                                 start=False, stop=(dc == 1)), r=[qk, cd], w=[ps_n],
                          sig=(dc == 1))
                live[(i, hl)] = ps_n

            def tail(i, hl):
                h = 2 * hh + hl
                ps_n = live.pop((i, hl))
                tcol = gtk[:, i * 8 + 4 + h:i * 8 + 4 + h + 1]
                hb = h2[hl]
                o = 0
                lnst = lnst_t[hl]
                rc = rec[:, hl * 4:hl * 4 + 1]
                fw.op(act, lambda e: e.activation(out=rc, in_=ps_n[:, 256:257], func=AF.Abs), r=[ps_n], w=[rec])
                fw.op(dve, lambda e: e.tensor_tensor(out=rc, in0=rc, in1=tcol, op=ALU.max), r=[rec, gtk], w=[rec])
                fw.op(dve, lambda e: e.reciprocal(out=rc, in_=rc), r=[rec], w=[rec])
                fw.op(dve, lambda e: e.scalar_tensor_tensor(out=hb[:, :], in0=ps_n[:, 0:256], scalar=rc,
                                                            in1=og[:, i, hl * 256:(hl + 1) * 256], op0=ALU.mult,
                                                            op1=ALU.mult), r=[ps_n, rec, og], w=[hb])
                st = lnst[:, o:o + 6]
                mv = lnst[:, o + 6:o + 8]
                var = lnst[:, o + 7:o + 8]
                fw.op(dve, lambda e: e.bn_stats(out=st, in_=hb[:, :]), r=[hb], w=[lnst])
                fw.op(dve, lambda e: e.bn_aggr(out=mv, in_=st), r=[lnst], w=[lnst])
                fw.op(pool, lambda e: e.tensor_scalar(out=var, in0=var, scalar1=4.0 * EPS, scalar2=1.0,
                                                      op0=ALU.add, op1=ALU.mult), r=[lnst], w=[lnst])
                fw.op(pool, lambda e: e.tensor_tensor(out=var, in0=var, in1=mhalf[:, 0:1], op=ALU.pow),
                      r=[lnst, mhalf], w=[lnst])
                lnb = lnb_t[hl]
                fw.op(pool, lambda e: e.tensor_scalar(out=lnb[:, :], in0=lnst[:, o + 6:o + 7], scalar1=var,
                                                      scalar2=-1.0, op0=ALU.mult, op1=ALU.mult),
                      r=[lnst], w=[lnb])

            def tail2(i, hl):
                hb = h2[hl]
                o = 0
                lnst = lnst_t[hl]
                lnb = lnb_t[hl]
                fw.op(act, lambda e: e.activation(out=hln[:, i, hl * 256:(hl + 1) * 256], in_=hb[:, :],
                                                  func=AF.Identity, scale=lnst[:, o + 7:o + 8], bias=lnb[:, 0:1]),
                      r=[hb, lnst, lnb], w=[hln])

            LG = 2 if OPT_LAG2 else 1
            for n in range(len(items) + LG):
                if n < len(items):
                    front(*items[n])
                if 1 <= n <= len(items):
                    tail(*items[n - 1])
                if n >= LG:
                    tail2(*items[n - LG])
            def ym_assemble(hh=hh):
                for i in range(4):
                    ps = psum()
                    pv = bfv(ps, 8)
                    ytmp = ytmps[i % 2]
                    for c in range(4):
                        fw.op(pe, lambda e: e.transpose(out=pv[:, c, :], in_=hln[:, i, c * 128:(c + 1) * 128],
                                                        identity=ident[:, :]), r=[hln, ident], w=[ps], sig=(c == 3))
                    fw.op(dve, lambda e: e.tensor_tensor(out=ytmp[:, :, :], in0=pv[:, 0:4, :],
                                                         in1=zs[:, :, i * 128:(i + 1) * 128], op=ALU.mult),
                          r=[ps, zs], w=[ytmp])
                    fw.op(pool, lambda e: e.tensor_tensor(out=yT[:, 4 * hh:4 * hh + 4, i * 128:(i + 1) * 128],
                                                          in0=ytmp[:, :, :], in1=skz[:, :, i * 128:(i + 1) * 128],
                                                          op=ALU.add), r=[ytmp, skz], w=[yT])

            if hh == 0:
                ym_assemble()
            else:
                ym_deferred = ym_assemble

        def rope_to(dsts, dst_deps, ps):
            fw.op(act, lambda e: e.activation(out=xb16[:, :], in_=ps[:, :], func=AF.Copy), r=[ps], w=[xb16])
            ps2 = psum()
            fw.op(pe, lambda e: e.matmul(ps2[:, :], lhsT=rm[:, :], rhs=xb16[:, :], start=True, stop=True),
                  r=[rm, xb16], w=[ps2])
            fw.op(dve, lambda e: e.tensor_tensor(out=tmpf[0][:, :], in0=ps[:, :], in1=ropeb[:, 0, :], op=ALU.mult),
                  r=[ps, ropeb], w=[tmpf[0]])
            fw.op(dve, lambda e: e.tensor_tensor(out=tmpf[1][:, :], in0=ps2[:, :], in1=ropeb[:, 1, :], op=ALU.mult),
                  r=[ps2, ropeb], w=[tmpf[1]])
            for (dst_ap, p0, p1) in dsts:
                fw.op(pool, lambda e: e.tensor_tensor(out=dst_ap, in0=tmpf[0][p0:p1, :], in1=tmpf[1][p0:p1, :],
                                                      op=ALU.add), r=[tmpf[0], tmpf[1]], w=dst_deps)

        fw.op(pool, lambda e: e.memset(qTa[64:128, :, 0, :], 0.0), w=[qTa])
        fw.op(pool, lambda e: e.memset(qTa[0:64, :, 1, :], 0.0), w=[qTa])
        wv = next_piece("in", 6)
        proj_fm(wv, lambda fb, ps: rope_to([(qTa[0:64, fb, 0, :], 0, 64), (qTa[64:128, fb, 1, :], 64, 128)],
                                           [qTa], ps))
        ym_deferred()
        wv = next_piece("in", 7)
        proj_fm(wv, lambda fb, ps: rope_to([(kTc[:, fb, T0:T0 + 512], 0, 128)], [kT_dep[fb][tb]], ps))
        wv = next_piece("in", 8)

        def cons_va(i, ps):
            fw.op(act, lambda e: e.activation(out=vc[:, 4 * tb + i, :, 0:64],
                                              in_=ps[:, :].rearrange("p (h d) -> p h d", h=8), func=AF.Copy),
                  r=[ps], w=[vc_dep[tb]])
        proj_tm(wv, cons_va)
        wv = next_piece("in", 9)

        def cons_za(fb, ps):
            th = tmpf[fb % 2]
            fw.op(act, lambda e: e.activation(out=th[:, :], in_=ps[:, :], func=AF.Tanh, scale=0.5), r=[ps], w=[th])
            fw.op(dve, lambda e: e.scalar_tensor_tensor(out=zas[:, fb, :], in0=th[:, :], scalar=1.0, in1=ps[:, :],
                                                        op0=ALU.add, op1=ALU.mult), r=[th, ps], w=[zas])
        proj_fm(wv, cons_za)

        pti = 0
        nI = 4 * tb + 4
        for pr in range(4):
            pos_ = [psum(pin=True), psum(pin=True)]
            povs_ = [View(p_[:, 0:260].rearrange("p (j d) -> p j d", j=4), p_.d) for p_ in pos_]
            aitems = [(I, hl) for I in range(nI) for hl in range(2)]
            alive = {}

            def a_qk(I, hl):
                hp = hl * 64
                J0 = max(I, 4 * tb)
                nq = 4 * tb + 4 - J0
                q0 = (J0 - 4 * tb) * 128
                ps_s = psum()
                fw.op(pe, lambda e: e.matmul(ps_s[:, 0:nq * 128], lhsT=kTc[:, pr, I * 128:(I + 1) * 128],
                                             rhs=qTa[:, pr, hl, q0:512], start=True, stop=True),
                      r=[kT_dep[pr][I // 4], qTa], w=[ps_s])
                alive[(I, hl)] = [ps_s, None]

            def a_exp(I, hl):
                nonlocal pti
                J0 = max(I, 4 * tb)
                nq = 4 * tb + 4 - J0
                ps_s = alive[(I, hl)][0]
                p = pT[pti % NPT]
                pti += 1
                fw.op(act, lambda e: e.activation(out=p[:, 0:nq * 128], in_=ps_s[:, 0:nq * 128], func=AF.Exp,
                                                  scale=0.125), r=[ps_s], w=[p])
                d0 = J0 - I
                meng = pool if (hl == 0 and I % 2 == 0) else dve
                fw.op(meng, lambda e: e.tensor_tensor(out=p[:, 0:nq * 128].rearrange("p (j t) -> p j t", j=nq),
                                                      in0=p[:, 0:nq * 128].rearrange("p (j t) -> p j t", j=nq),
                                                      in1=msk[:, d0:d0 + nq, :], op=ALU.mult), r=[p, msk], w=[p])
                alive[(I, hl)][1] = p

            def a_pv(I, hl):
                h = 2 * pr + hl
                J0 = max(I, 4 * tb)
                nq = 4 * tb + 4 - J0
                p = alive.pop((I, hl))[1]
                for jj in range(nq):
                    jl = J0 + jj - 4 * tb
                    fw.op(pe, lambda e: e.matmul(povs_[hl][:, jl, :], lhsT=p[:, jj * 128:(jj + 1) * 128],
                                                 rhs=vc[:, I, h, :], start=(I == 0 and jj == 0),
                                                 stop=(I == nI - 1), skip_group_check=True),
                          r=[p, vc_dep[I // 4]], w=[pos_[hl]], sig=(jj == nq - 1))

            L1, L2 = 2, 4
            for n in range(len(aitems) + L2):
                if n < len(aitems):
                    a_qk(*aitems[n])
                if 0 <= n - L1 < len(aitems):
                    a_exp(*aitems[n - L1])
                if 0 <= n - L2 < len(aitems):
                    a_pv(*aitems[n - L2])
            for hl in range(2):
                h = 2 * pr + hl
                po, pov = pos_[hl], povs_[hl]
                fw.op(dve, lambda e: e.reciprocal(out=rec[:, 0:4], in_=pov[:, :, 64:65].rearrange("p j o -> p (j o)")),
                      r=[po], w=[rec])
                fw.op(dve, lambda e: e.tensor_tensor(out=oa_tok[:, :, h * 64:(h + 1) * 64], in0=pov[:, :, 0:64],
                                                     in1=rec[:, 0:4].unsqueeze(2).to_broadcast([128, 4, 64]),
                                                     op=ALU.mult), r=[po, rec], w=[oa_tok])
                unpin(po)
        for i in range(4):
            ps = psum()
            pv = bfv(ps, 8)
            for c in range(4):
                fw.op(pe, lambda e: e.transpose(out=pv[:, c, :], in_=oa_tok[:, i, c * 128:(c + 1) * 128],
                                                identity=ident[:, :]), r=[oa_tok, ident], w=[ps], sig=(c == 3))
            fw.op(dve, lambda e: e.scalar_tensor_tensor(out=yT[:, 8:12, i * 128:(i + 1) * 128], in0=pv[:, 0:4, :],
                                                        scalar=0.5, in1=zas[:, :, i * 128:(i + 1) * 128],
                                                        op0=ALU.mult, op1=ALU.mult), r=[ps, zas], w=[yT])

        wv = next_piece("in", 10)

        def cons_qx(fb, ps):
            fw.op(act, lambda e: e.activation(out=qxT[:, fb, :], in_=ps[:, :], func=AF.Copy), r=[ps], w=[qxT])
        proj_fm(wv, cons_qx)
        wv = next_piece("in", 11)

        def cons_zx(fb, ps):
            th = tmpf[fb % 2]
            fw.op(act, lambda e: e.activation(out=th[:, :], in_=ps[:, :], func=AF.Tanh, scale=0.5), r=[ps], w=[th])
            fw.op(dve, lambda e: e.scalar_tensor_tensor(out=zxs[:, fb, :], in0=th[:, :], scalar=1.0, in1=ps[:, :],
                                                        op0=ALU.add, op1=ALU.mult), r=[th, ps], w=[zxs])
        proj_fm(wv, cons_zx)
        xlive = {}

        def x_qk(h):
            nonlocal pti
            for mi in range(2):
                ps_s = psum()
                fw.op(pe, lambda e: e.matmul(ps_s[:, :], lhsT=kxT[:, h, mi * 128:(mi + 1) * 128], rhs=qxT[:, h, :],
                                             start=True, stop=True), r=[kxT, qxT], w=[ps_s])
                p = pT[pti % NPT]
                pti += 1
                fw.op(act, lambda e: e.activation(out=p[:, :], in_=ps_s[:, :], func=AF.Exp,
                                                  scale=float(128 ** -0.5)), r=[ps_s], w=[p])
                xlive[(h, mi)] = p

        def x_pv(h):
            pos = [psum(pin=True), psum(pin=True)]
            povs = [View(p_[:, 0:258].rearrange("p (j d) -> p j d", j=2), p_.d) for p_ in pos]
            for mi in range(2):
                p = xlive.pop((h, mi))
                for j in range(4):
                    fw.op(pe, lambda e: e.matmul(povs[j // 2][:, j % 2, :], lhsT=p[:, j * 128:(j + 1) * 128],
                                                 rhs=vx[:, mi, h, :], start=(mi == 0 and j % 2 == 0),
                                                 stop=(mi == 1), skip_group_check=True),
                          r=[p, vx], w=[pos[j // 2]], sig=(j % 2 == 1))
            for b2 in range(2):
                fw.op(dve, lambda e: e.reciprocal(out=rec[:, 4:6],
                                                  in_=povs[b2][:, :, 128:129].rearrange("p j o -> p (j o)")),
                      r=[pos[b2]], w=[rec])
                fw.op(dve, lambda e: e.tensor_tensor(out=ox_tok[:, 2 * b2:2 * b2 + 2, h * 128:(h + 1) * 128],
                                                     in0=povs[b2][:, :, 0:128],
                                                     in1=rec[:, 4:6].unsqueeze(2).to_broadcast([128, 2, 128]),
                                                     op=ALU.mult), r=[pos[b2], rec], w=[ox_tok])
                unpin(pos[b2])

        for n in range(5):
            if n < 4:
                x_qk(n)
            if n >= 1:
                x_pv(n - 1)
        for i in range(4):
            ps = psum()
            pv = bfv(ps, 8)
            for c in range(4):
                fw.op(pe, lambda e: e.transpose(out=pv[:, c, :], in_=ox_tok[:, i, c * 128:(c + 1) * 128],
                                                identity=ident[:, :]), r=[ox_tok, ident], w=[ps], sig=(c == 3))
            fw.op(dve, lambda e: e.scalar_tensor_tensor(out=yT[:, 12:16, i * 128:(i + 1) * 128], in0=pv[:, 0:4, :],
                                                        scalar=0.5, in1=zxs[:, :, i * 128:(i + 1) * 128],
                                                        op0=ALU.mult, op1=ALU.mult), r=[ps, zxs], w=[yT])
        if debug:
            fw.dma(dbg_d[s, tb], yT[:, :, :], r=[yT])

        def emit_next_A():
            if bi + 1 < len(blocks):
                ns_, ntb_ = blocks[bi + 1]
                if ntb_ == 0:
                    seq_setup(ns_)
                stage_A(ns_, ntb_ * 512)
        if OPT_EARLY_A:
            emit_next_A()
        obanks = [[psum() for _ in range(2)] for _ in range(4)]
        for q in range(4):
            wv = next_piece("out", q)
            for j in range(4):
                for half in range(2):
                    for kc in range(4):
                        c = 4 * q + kc
                        fw.op(pe, lambda e: e.matmul(obanks[j][half][:, :], lhsT=yT[:, c, j * 128:(j + 1) * 128],
                                                     rhs=wv[:, kc, half * 512:(half + 1) * 512],
                                                     start=(c == 0), stop=(c == 15)),
                              r=[yT, wv], w=[obanks[j][half]], sig=(kc == 3))
        for j in range(4):
            xt = next_xt()
            fw.dma(xt[:, :], x_d[s, T0 + j * 128:T0 + (j + 1) * 128, :], w=[xt], Q=pool)
            for half in range(2):
                fw.op(dve, lambda e: e.tensor_tensor(out=xt[:, half * 512:(half + 1) * 512],
                                                     in0=obanks[j][half][:, :],
                                                     in1=xt[:, half * 512:(half + 1) * 512], op=ALU.add),
                      r=[obanks[j][half], xt], w=[xt])
            fw.op(act, lambda e: e.activation(out=junk[:, :], in_=xt[:, :], func=AF.Square, accum_out=stat[:, 2:3]),
                  r=[xt], w=[junk, stat])
            rms_rstd(stat[:, 2:3], stat[:, 3:4])
            ot = xt
            fw.op(dve, lambda e: e.scalar_tensor_tensor(out=ot[:, :], in0=xt[:, :], scalar=stat[:, 3:4],
                                                        in1=gfinal_row[:, :], op0=ALU.mult, op1=ALU.mult),
                  r=[xt, stat, gfinal_row], w=[ot])
            fw.dma(out_d[s, T0 + j * 128:T0 + (j + 1) * 128, :], ot[:, :], r=[ot], Q=act)
        if not OPT_EARLY_A:
            emit_next_A()

    fw.finish()
    stats = dict(sbuf_free=nc.sbuf_bytes_remaining, pe=pe.ninstr, act=act.ninstr, dve=dve.ninstr, pool=pool.ninstr, waits=fw.nwaits)
    fw.close()
    return nc, stats


_CACHE = {}


def _host_inputs(inp):
    f = lambda a: np.ascontiguousarray(np.asarray(a, dtype=np.float32))
    c = _consts()
    shared = dict(c)
    shared["w_in"] = f(inp["w_in"][0])
    shared["w_out"] = f(inp["w_out"][0])
    shared["w_mem_kv"] = f(inp["w_mem_kv"][0])
    for nm, key in (("bdq", "w_q_blk"), ("bdk", "w_k_blk"), ("bdv", "w_v_blk")):
        bd = _blockdiag(f(inp[key][0]))
        shared[nm] = bd
        shared["bdt" + nm[2]] = np.ascontiguousarray(bd.transpose(0, 2, 1))
    shared["wg"] = np.ascontiguousarray(f(inp["w_gate"][0]).reshape(24, 128, 8).transpose(1, 0, 2))
    pf = np.zeros((128, 72), np.float32)
    cw = f(inp["conv_w"][0])
    for j in range(4):
        pf[:, j * 8:(j + 1) * 8] = cw[j].reshape(8, 128).T
    pf[:, 32:40] = f(inp["conv_b"][0]).reshape(8, 128).T
    pf[:, 40:48] = f(inp["g_head"][0]).reshape(8, 128).T
    pf[:, 48:56] = f(inp["skip"][0]).reshape(8, 128).T
    pf[:, 56:64] = f(inp["g_mem"][0]).reshape(8, 128).T
    pf[:, 64:72] = f(inp["g_norm"][0]).reshape(8, 128).T
    shared["pf"] = pf
    shared["bg"] = np.ascontiguousarray(f(inp["b_gate"][0]).reshape(2, 4).T)
    shared["gfinal_row"] = np.ascontiguousarray(np.broadcast_to(f(inp["g_final"])[None, :], (128, D)))
    shared["cb_row"] = f(inp["conv_b"][0]).reshape(1, D)
    return shared


def kernel(**inputs):
    x = np.asarray(inputs["x"], dtype=np.float32)
    mem = np.asarray(inputs["mem"], dtype=np.float32)
    B = x.shape[0]
    nseq = B // NCORES
    if "nc" not in _CACHE:
        _CACHE["nc"] = build(nseq=nseq, nblk=4)[0]
    nc = _CACHE["nc"]
    shared = _host_inputs(inputs)
    in_maps = []
    for c in range(NCORES):
        m = dict(shared)
        m["x"] = np.ascontiguousarray(x[c * nseq:(c + 1) * nseq])
        m["mem"] = np.ascontiguousarray(mem[c * nseq:(c + 1) * nseq])
        in_maps.append(m)
    res = run_bass_kernel_spmd(nc, in_maps, core_ids=list(range(NCORES)))
    out = np.concatenate([np.asarray(r["out"]) for r in res.results], axis=0)
    return out.astype(np.float32)
```
